# Optimizing a Trainium2 kernel written in Bass

```python
import math
import jax, jax.numpy as jnp
from jax import lax
import numpy as np


D_MODEL = 1024
BATCH = 4
SEQ = 8192
DEPTH = 2

GRID_W = 64
CTX_LEN = 256
DA_HEADS = 4
DA_HEAD_DIM = 64
DA_QK_W = DA_HEADS * 2 * DA_HEAD_DIM
DA_V_W = DA_HEADS * 2 * DA_HEAD_DIM
CONV_C = 512
CONV_K = 31
GATE_W = 2 * D_MODEL
Q_END = DA_QK_W
K_END = Q_END + DA_QK_W
V_END = K_END + DA_V_W
U_END = V_END + 2 * CONV_C
IN_W = U_END + GATE_W
ROPE_AXIS_DIM = DA_HEAD_DIM // 2
ROPE_BASE = 10000.0
Q_BLOCK = 128
N_EXPERTS = 16
N_GROUPS = 4
EXPERTS_PER_GROUP = N_EXPERTS // N_GROUPS
TOP_K = 2
D_EXPERT = 512
N_MOD = 6
DEEPNORM_ALPHA = (2 * DEPTH) ** 0.25
DEEPNORM_BETA = (8 * DEPTH) ** -0.25
EPS = 1e-5

kernel_name = 'hybrid_diffattn_conformer_grouped_moe_dit'


def layer_norm(x, g, b):
    xf = x.astype(jnp.float32)
    mu = jnp.mean(xf, -1, keepdims=True)
    var = jnp.mean(jnp.square(xf - mu), -1, keepdims=True)
    return ((xf - mu) * lax.rsqrt(var + EPS) * g + b).astype(x.dtype)


def modulate(x, shift, scale):
    return x * (1.0 + scale) + shift


def axial_rope(rows):
    row = jnp.repeat(jnp.arange(rows, dtype=jnp.float32), GRID_W)
    col = jnp.tile(jnp.arange(GRID_W, dtype=jnp.float32), rows)
    inv_freq = ROPE_BASE ** (-jnp.arange(0, ROPE_AXIS_DIM, 2, dtype=jnp.float32) / ROPE_AXIS_DIM)
    ang = jnp.concatenate([row[:, None] * inv_freq, col[:, None] * inv_freq], -1)
    return jnp.cos(ang), jnp.sin(ang)


def apply_rope(x, cos, sin):
    xp = x.astype(jnp.float32).reshape(*x.shape[:-1], DA_HEAD_DIM // 2, 2)
    c = cos[None, :, None, None, :]
    s = sin[None, :, None, None, :]
    x0, x1 = xp[..., 0], xp[..., 1]
    out = jnp.stack([x0 * c - x1 * s, x0 * s + x1 * c], -1)
    return out.reshape(x.shape).astype(x.dtype)


def qk_heads(p):
    return p.reshape(*p.shape[:-1], DA_HEADS, 2, DA_HEAD_DIM)


def v_heads(p):
    return p.reshape(*p.shape[:-1], DA_HEADS, 2 * DA_HEAD_DIM)


def diff_attend(q, k, v, lam):
    s = jnp.einsum('bqhmd,bkhmd->bhmqk', q, k).astype(jnp.float32) * (DA_HEAD_DIM ** -0.5)
    a = jax.nn.softmax(s, axis=-1)
    a = a[:, :, 0] - lam * a[:, :, 1]
    return jnp.einsum('bhqk,bkhe->bqhe', a.astype(v.dtype), v)


def latent_diff_attention(q, k, v, lam):
    b, n = q.shape[:2]
    n_blocks = n // Q_BLOCK
    q_blocks = jnp.moveaxis(q.reshape(b, n_blocks, Q_BLOCK, *q.shape[2:]), 1, 0)
    out = lax.map(lambda qb: diff_attend(qb, k, v, lam), q_blocks)
    return jnp.moveaxis(out, 0, 1).reshape(b, n, DA_HEADS, 2 * DA_HEAD_DIM)


def diff_heads_out(o, g, lam_init, w_o):
    of = o.astype(jnp.float32)
    of = of * lax.rsqrt(jnp.mean(jnp.square(of), -1, keepdims=True) + EPS) * g * (1.0 - lam_init)
    return of.reshape(*o.shape[:-2], DA_V_W).astype(o.dtype) @ w_o


def conformer_conv(u, conv_w, conv_b, ln_g, ln_b, w_o):
    a, gate = jnp.split(u, 2, axis=-1)
    y = a * jax.nn.sigmoid(gate)
    y = lax.conv_general_dilated(
        y, conv_w[:, None, :], window_strides=(1,),
        padding=((CONV_K // 2, CONV_K // 2),),
        dimension_numbers=('NWC', 'WIO', 'NWC'),
        feature_group_count=CONV_C) + conv_b
    y = jax.nn.silu(layer_norm(y, ln_g, ln_b))
    return y @ w_o


def mixer_merge(p, attn, lam_init, subln_g, w_attn_o, conv_w, conv_b, conv_ln_g, conv_ln_b, w_conv_o, w_out):
    att = diff_heads_out(attn, subln_g, lam_init, w_attn_o)
    cnv = conformer_conv(p[..., V_END:U_END], conv_w, conv_b, conv_ln_g, conv_ln_b, w_conv_o)
    g_att, g_cnv = jnp.split(p[..., U_END:], 2, axis=-1)
    return (jax.nn.sigmoid(g_att) * att + jax.nn.sigmoid(g_cnv) * cnv) @ w_out


def grouped_moe(h, w_router, b_router, w_gate, w_up, w_down):
    probs = jax.nn.softmax((h @ w_router + b_router).astype(jnp.float32), axis=-1)
    grouped = probs.reshape(*probs.shape[:-1], N_GROUPS, EXPERTS_PER_GROUP)
    group_score = lax.top_k(grouped, TOP_K)[0].sum(-1)
    group_sel = jnp.argmax(group_score, -1)[..., None] == jnp.arange(N_GROUPS)
    expert_ok = jnp.repeat(group_sel, EXPERTS_PER_GROUP, axis=-1)
    top_p, top_i = lax.top_k(jnp.where(expert_ok, probs, -1.0), TOP_K)
    top_w = top_p / top_p.sum(-1, keepdims=True)
    gates = jnp.einsum('...ke,...k->...e', jax.nn.one_hot(top_i, N_EXPERTS, dtype=jnp.float32), top_w).astype(h.dtype)
    out = jnp.zeros_like(h)
    for e in range(N_EXPERTS):
        y = (jax.nn.silu(h @ w_gate[e]) * (h @ w_up[e])) @ w_down[e]
        out = out + gates[..., e:e + 1] * y
    return out


def setup_inputs(seed: int = 0) -> dict:
    key = jax.random.key(seed)
    ks = jax.random.split(key, 28)

    def nrm(i, shape, scale):
        return scale * jax.random.normal(ks[i], shape, jnp.float32)

    D, L = D_MODEL, DEPTH
    return {
        'x': nrm(0, (BATCH, SEQ, D), 1.0),
        'c': nrm(1, (BATCH, D), 1.0),
        'ctx': nrm(2, (BATCH, CTX_LEN, D), 1.0),
        'c_ctx': nrm(3, (D,), 1.0),
        'w_ada': nrm(4, (L, D, N_MOD * D), 0.5 * D ** -0.5),
        'b_ada': nrm(5, (L, N_MOD * D), 0.02),
        'w_in': nrm(6, (L, D, IN_W), D ** -0.5),
        'lam_q1': nrm(7, (L, DA_HEAD_DIM), 0.1),
        'lam_k1': nrm(8, (L, DA_HEAD_DIM), 0.1),
        'lam_q2': nrm(9, (L, DA_HEAD_DIM), 0.1),
        'lam_k2': nrm(10, (L, DA_HEAD_DIM), 0.1),
        'subln_g': 1.0 + nrm(11, (L, 2 * DA_HEAD_DIM), 0.02),
        'w_attn_o': nrm(12, (L, DA_V_W, D), DA_V_W ** -0.5),
        'conv_w': nrm(13, (L, CONV_K, CONV_C), CONV_K ** -0.5),
        'conv_b': nrm(14, (L, CONV_C), 0.02),
        'conv_ln_g': 1.0 + nrm(15, (L, CONV_C), 0.02),
        'conv_ln_b': nrm(16, (L, CONV_C), 0.02),
        'w_conv_o': nrm(17, (L, CONV_C, D), CONV_C ** -0.5),
        'w_out': nrm(18, (L, D, D), DEEPNORM_BETA * D ** -0.5),
        'ln1_g': 1.0 + nrm(19, (L, D), 0.02),
        'ln1_b': nrm(20, (L, D), 0.02),
        'w_router': nrm(21, (D, N_EXPERTS), D ** -0.5),
        'b_router': nrm(22, (N_EXPERTS,), 0.01),
        'w_e_gate': nrm(23, (L, N_EXPERTS, D, D_EXPERT), D ** -0.5),
        'w_e_up': nrm(24, (L, N_EXPERTS, D, D_EXPERT), D ** -0.5),
        'w_e_down': nrm(25, (L, N_EXPERTS, D_EXPERT, D), DEEPNORM_BETA * D_EXPERT ** -0.5),
        'ln2_g': 1.0 + nrm(26, (L, D), 0.02),
        'ln2_b': nrm(27, (L, D), 0.02),
    }


def reference(x, c, ctx, c_ctx, w_ada, b_ada, w_in, lam_q1, lam_k1, lam_q2, lam_k2, subln_g,
              w_attn_o, conv_w, conv_b, conv_ln_g, conv_ln_b, w_conv_o, w_out, ln1_g, ln1_b,
              w_router, b_router, w_e_gate, w_e_up, w_e_down, ln2_g, ln2_b):
    n = x.shape[1]
    ROWS = n // GRID_W
    cos, sin = axial_rope(ROWS)
    cx = ctx
    for l in range(DEPTH):
        last = l == DEPTH - 1
        lam_init = 0.8 - 0.6 * math.exp(-0.3 * l)
        lam = (jnp.exp(jnp.sum(lam_q1[l] * lam_k1[l]).astype(jnp.float32))
               - jnp.exp(jnp.sum(lam_q2[l] * lam_k2[l]).astype(jnp.float32)) + lam_init)
        mod_x = jnp.split((jax.nn.silu(c) @ w_ada[l] + b_ada[l])[:, None, :], N_MOD, axis=-1)
        mod_c = jnp.split(jax.nn.silu(c_ctx) @ w_ada[l] + b_ada[l], N_MOD, axis=-1)
        w_in_l = w_in[l]
        mix_args = (lam_init, subln_g[l], w_attn_o[l], conv_w[l], conv_b[l], conv_ln_g[l],
                    conv_ln_b[l], w_conv_o[l], w_out[l])
        moe_args = (w_router, b_router, w_e_gate[l], w_e_up[l], w_e_down[l])

        hx = modulate(x, mod_x[0], mod_x[1])
        hc = modulate(cx, mod_c[0], mod_c[1])
        px = hx @ w_in_l
        pc = hc @ (w_in_l[:, :V_END] if last else w_in_l)
        kc = qk_heads(pc[..., Q_END:K_END])
        vc = v_heads(pc[..., K_END:V_END])
        qx = apply_rope(qk_heads(px[..., :Q_END]), cos, sin)
        kx = apply_rope(qk_heads(px[..., Q_END:K_END]), cos, sin)
        vx = v_heads(px[..., K_END:V_END])
        attn_x = latent_diff_attention(qx, jnp.concatenate([kc, kx], 1),
                                       jnp.concatenate([vc, vx], 1), lam)
        mix_x = mixer_merge(px, attn_x, *mix_args)

        if not last:
            attn_c = diff_attend(qk_heads(pc[..., :Q_END]), kc, vc, lam)
            mix_c = mixer_merge(pc, attn_c, *mix_args)
            cx = layer_norm(DEEPNORM_ALPHA * cx + mod_c[2] * mix_c, ln1_g[l], ln1_b[l])
            hc = modulate(cx, mod_c[3], mod_c[4])
            cx = layer_norm(DEEPNORM_ALPHA * cx + mod_c[5] * grouped_moe(hc, *moe_args),
                            ln2_g[l], ln2_b[l])

        x = layer_norm(DEEPNORM_ALPHA * x + mod_x[2] * mix_x, ln1_g[l], ln1_b[l])
        hx = modulate(x, mod_x[3], mod_x[4])
        x = layer_norm(DEEPNORM_ALPHA * x + mod_x[5] * grouped_moe(hx, *moe_args),
                       ln2_g[l], ln2_b[l])
    return x
```

```python
import bisect
import contextlib
import math
import numpy as np
import concourse.bass as bass
import concourse.mybir as mybir
from concourse.bass_utils import run_bass_kernel_spmd

F32 = mybir.dt.float32
BF16 = mybir.dt.bfloat16
U8 = mybir.dt.uint8
AF = mybir.ActivationFunctionType
ALU = mybir.AluOpType
AX = mybir.AxisListType

ENGS = ('pe', 'act', 'dve', 'pool', 'sp')

D = 1024
NLAT = 4096
NCTX = 256
TOK = NLAT + NCTX
NE = 16
DE = 512
INW = 5632
ALPHA = 4.0 ** 0.25
EPS = 1e-5
BROWS = 8256
KROWS = 4096
VROWS = 4128


class Res:
    __slots__ = ('w', 'r', 'name')

    def __init__(self, name=''):
        self.w = {}
        self.r = {}
        self.name = name


class Sched:
    def __init__(self, nc):
        self.nc = nc
        self.progs = {e: [] for e in ENGS}
        self.cnt = {e: 0 for e in ENGS}
        self.known = {e: {} for e in ENGS}
        self.snaps = {e: ([0], [{}]) for e in ENGS}
        self.dma_keys = []
        self.cc_keys = set()
        self.targets = {e: set() for e in ENGS}

    def dma_key(self, name, cc=False):
        k = 'd_' + name
        assert k not in self.cnt
        self.cnt[k] = 0
        self.dma_keys.append(k)
        if cc:
            self.cc_keys.add(k)
        return k

    def _merge_known(self, eng, key, val):
        kn = self.known[eng]
        if kn.get(key, 0) < val:
            kn[key] = val
        if key in self.snaps:
            counts, dicts = self.snaps[key]
            i = bisect.bisect_right(counts, val) - 1
            for k2, v2 in dicts[i].items():
                if kn.get(k2, 0) < v2:
                    kn[k2] = v2

    def _add_waits(self, eng, need):
        kn = self.known[eng]
        waits = [(k, v) for k, v in need.items() if kn.get(k, 0) < v]
        for k, v in waits:
            self._merge_known(eng, k, v)
            if k in self.targets:
                self.targets[k].add(v)
        if waits:
            counts, dicts = self.snaps[eng]
            counts.append(self.cnt[eng] + 1)
            dicts.append(dict(kn))
        return waits

    def op(self, eng, fn, reads=(), writes=(), dma=None):
        need = {}

        def add(clk, same_ok):
            if clk is None:
                return
            k, v = clk
            if k == eng and (eng == 'pe' or not same_ok):
                return
            if need.get(k, 0) < v:
                need[k] = v
        for r in reads:
            for k, v in r.w.items():
                add((k, v), True)
        for w in writes:
            for k, v in w.w.items():
                add((k, v), True)
            for k, v in w.r.items():
                add((k, v), False)
        waits = self._add_waits(eng, need)
        if dma is None:
            self.cnt[eng] += 1
            clk = (eng, self.cnt[eng])
        else:
            self.cnt[dma] += 1
            clk = (dma, self.cnt[dma])
        self.progs[eng].append((fn, waits, clk))
        for r in reads:
            if r.r.get(clk[0], 0) < clk[1]:
                r.r[clk[0]] = clk[1]
        for w in writes:
            if w.w.get(clk[0], 0) < clk[1]:
                w.w[clk[0]] = clk[1]
            w.r = {}
        return clk

    def barrier(self):
        tot = dict(self.cnt)
        for e in ENGS:
            need = {k: v for k, v in tot.items() if k != e and v > 0}
            waits = self._add_waits(e, need)
            if waits:
                self.progs[e].append((None, waits, None))

    def emit(self, final_engine='sp'):
        nc = self.nc
        tot = dict(self.cnt)
        need = {k: v for k, v in tot.items() if k != final_engine and v > 0}
        fw = self._add_waits(final_engine, need)
        if fw:
            self.progs[final_engine].append((None, fw, None))
        tl = {e: sorted(self.targets[e]) for e in ENGS}

        def semval(k, v):
            if k in tl:
                i = bisect.bisect_left(tl[k], v)
                assert i < len(tl[k]) and tl[k][i] == v, (k, v)
                return i + 1
            if k in self.cc_keys:
                return v
            return 16 * v
        keys = list(ENGS) + self.dma_keys
        with contextlib.ExitStack() as st:
            sems = {k: st.enter_context(nc.semaphore('s_' + k)) for k in keys}
            block = st.enter_context(nc.Block())
            handles = {'pe': block.tensor, 'act': block.scalar, 'dve': block.vector,
                       'pool': block.gpsimd, 'sp': block.sync}

            def mk(ename):
                prog = self.progs[ename]
                tset = self.targets[ename]

                def body(e):
                    for fn, waits, clk in prog:
                        for k, v in waits:
                            e.wait_ge(sems[k], semval(k, v))
                        if fn is None:
                            continue
                        ins = fn(e)
                        if clk[0] == ename:
                            if clk[1] in tset:
                                ins.then_inc(sems[ename], 1)
                        elif clk[0] in self.cc_keys:
                            ins.then_inc(sems[clk[0]])
                        else:
                            ins.then_inc(sems[clk[0]], 16)
                return body
            for ename in ENGS:
                handles[ename](mk(ename))
        self.stats = {e: (len(self.progs[e]), len(tl[e])) for e in ENGS}


class Arena:
    def __init__(self, ap_u8, size):
        self.t = ap_u8
        self.size = size
        self.off = 0
        self.marks = []
        self.peak = 0

    def alloc(self, shape_free, dtype, parts=128):
        esz = {F32: 4, BF16: 2}[dtype]
        n = int(np.prod(shape_free))
        nbytes = n * esz
        off = (self.off + 63) // 64 * 64
        assert off + nbytes <= self.size, f'SBUF arena overflow: {off}+{nbytes}>{self.size}'
        self.off = off + nbytes
        self.peak = max(self.peak, self.off)
        ap = self.t[0:parts, off:off + nbytes].bitcast(dtype)
        if len(shape_free) > 1:
            names = ' '.join(f'a{i}' for i in range(len(shape_free)))
            kw = {f'a{i}': int(s) for i, s in enumerate(shape_free)}
            ap = ap.rearrange(f'p ({names}) -> p {names}', **kw)
        return ap

    def mark(self):
        self.marks.append(self.off)

    def release(self):
        self.off = self.marks.pop()


ARENA_BYTES = 200 * 1024


def build_program(dbg=False, n_layers=2, stop=None):
    nc = bass.Bass("TRN2", target_bir_lowering=False)

    def din(name, shape, dt=F32):
        return nc.dram_tensor(name, list(shape), dt, kind="ExternalInput").ap()

    x_own = din("x_own", [NLAT, D])
    ctxb = din("ctxb", [NCTX, D])
    ccT = din("ccT", [128, 16])
    identd = din("identd", [128, 128])
    w_ada = din("w_ada", [2, D, 6 * D])
    b_adaT = din("b_adaT", [128, 96])
    w_in_r = din("w_in_r", [2, D, INW])
    ropeC = din("ropeC", [128, NLAT])
    ropeS = din("ropeS", [128, NLAT])
    lamv = din("lamv", [2, 256])
    sublng = din("sublng", [2, 128])
    w_attn_o = din("w_attn_o", [2, 512, D])
    w_conv_o = din("w_conv_o", [2, 512, D])
    w_out = din("w_out", [2, D, D])
    convw = din("convw", [128, 2 * 4 * 31])
    convv = din("convv", [128, 2 * 3 * 4])
    lnv = din("lnv", [2, 4 * D])
    w_router_r = din("w_router_r", [128, 8 * 16])
    b_router = din("b_router", [1, 16])
    halo_sel = din("halo_sel", [128, 2])
    nexp = 1 if stop in ('p0', 'A', 'X', 'B', 'C') else NE
    w_e_gate = din("w_e_gate", [2, nexp, D, DE])
    w_e_up = din("w_e_up", [2, nexp, D, DE])
    w_e_down = din("w_e_down", [2, nexp, DE, D])
    out = nc.dram_tensor("out", [NLAT, D], F32, kind="ExternalOutput").ap()
    kind_dbg = "ExternalOutput" if dbg else "Internal"
    xs1 = nc.dram_tensor("xs1", [TOK, D], F32, kind=kind_dbg).ap()
    xs2 = nc.dram_tensor("xs2", [TOK, D], F32, kind=kind_dbg).ap()
    gsc = nc.dram_tensor("gsc", [128, 16, TOK], BF16).ap()
    qsc = nc.dram_tensor("qsc", [4, 128, NLAT], BF16).ap()
    ysc = nc.dram_tensor("ysc", [128, 4, NLAT + 32], BF16).ap()
    ycsc = nc.dram_tensor("ycsc", [128, 4, NCTX + 32], BF16).ap()
    ansc = nc.dram_tensor("ansc", [128, 4, TOK], BF16).ap()
    wc_g = nc.dram_tensor("wc_g", [NE, 128, 8 * 512], BF16).ap()
    wc_u = nc.dram_tensor("wc_u", [NE, 128, 8 * 512], BF16).ap()
    wc_d = nc.dram_tensor("wc_d", [NE, 128, 4 * D], BF16).ap()
    wc_3 = nc.dram_tensor("wc_3", [128, 16 * D], BF16).ap()
    CH_ROWS = [1024] * 8 + [32]
    bounce_t = [[nc.dram_tensor(f"bounce{l}_{c}", [CH_ROWS[c], 512], BF16) for c in range(9)] for l in range(2)]
    gath_t = [[nc.dram_tensor(f"gath{l}_{c}", [2 * CH_ROWS[c], 512], BF16) for c in range(9)] for l in range(2)]

    S = Sched(nc)
    st = contextlib.ExitStack()
    with st:
        arena_t = st.enter_context(nc.sbuf_tensor("arena", [128, ARENA_BYTES], U8))
        A = Arena(arena_t.ap() if hasattr(arena_t, 'ap') else arena_t[:, :], ARENA_BYTES)
        PSh = [st.enter_context(nc.psum_tensor(f"ps{i}", [128, 512], F32)) for i in range(4)]
        SCh = [st.enter_context(nc.psum_tensor(f"sc{i}", [128, 1024], F32)) for i in range(2)]
        PS = [p[:, :] for p in PSh]
        SC = [p[:, :] for p in SCh]
        for sc_ in SC:
            PS.append(sc_[:, 0:512])
            PS.append(sc_[:, 512:1024])
        RPS = [Res(f'ps{i}') for i in range(8)]

        def mm(o, lhsT, rhs, start, stop, reads, writes, tp=None, skip=False):
            if tp is None:
                S.op('pe', lambda e: e.matmul(o, lhsT=lhsT, rhs=rhs, start=start, stop=stop,
                                              skip_group_check=skip), reads, writes)
            else:
                S.op('pe', lambda e: e.matmul(o, lhsT=lhsT, rhs=rhs, start=start, stop=stop,
                                              skip_group_check=skip, tile_position=tp), reads, writes)

        def tr(o, in_, reads, writes):
            S.op('pe', lambda e: e.transpose(out=o, in_=in_, identity=identf), reads + [R_const], writes)

        def act(o, in_, func, reads, writes, bias=None, scale=None, accum=None):
            kw = {}
            if bias is not None:
                kw['bias'] = bias
            if scale is not None:
                kw['scale'] = scale
            if accum is not None:
                kw['accum_out'] = accum
            S.op('act', lambda e: e.activation(out=o, in_=in_, func=func, **kw), reads, writes)

        def tt(eng, o, a, b, op, reads, writes):
            S.op(eng, lambda e: e.tensor_tensor(out=o, in0=a, in1=b, op=op), reads, writes)

        def ts(eng, o, a, s1, op0, reads, writes, s2=None, op1=None):
            if op1 is None:
                S.op(eng, lambda e: e.tensor_scalar(out=o, in0=a, scalar1=s1, scalar2=None, op0=op0), reads, writes)
            else:
                S.op(eng, lambda e: e.tensor_scalar(out=o, in0=a, scalar1=s1, scalar2=s2, op0=op0, op1=op1),
                     reads, writes)

        def stt(eng, o, a, s, b, op0, op1, reads, writes):
            S.op(eng, lambda e: e.scalar_tensor_tensor(out=o, in0=a, scalar=s, in1=b, op0=op0, op1=op1),
                 reads, writes)

        def cp(eng, o, a, reads, writes):
            if eng == 'act':
                S.op(eng, lambda e: e.activation(out=o, in_=a, func=AF.Copy), reads, writes)
            else:
                S.op(eng, lambda e: e.tensor_copy(out=o, in_=a), reads, writes)

        def memset(eng, o, v, writes):
            S.op(eng, lambda e: e.memset(o, v), [], writes)

        def recip(o, a, reads, writes):
            S.op('dve', lambda e: e.reciprocal(out=o, in_=a), reads, writes)

        dma_ctr = [0]

        def dma(eng, o, i, reads, writes, key=None):
            if key is None:
                key = KP_misc.get(eng)
            elif isinstance(key, KeyPool):
                key = key.get(eng)
            S.op(eng, lambda e: e.dma_start(out=o, in_=i), reads, writes, dma=key)

        class KeyPool:
            def __init__(self, name, n):
                self.name = name
                self.n = n
                self.keys = {}
                self.i = {}

            def next(self):
                return self

            def get(self, eng):
                if eng not in self.keys:
                    self.keys[eng] = [S.dma_key(f'{self.name}_{eng}{i}') for i in range(self.n)]
                    self.i[eng] = 0
                k = self.keys[eng][self.i[eng] % self.n]
                self.i[eng] += 1
                return k
        KP_ld = KeyPool('ld', 16)
        KP_st = KeyPool('st', 10)
        KP_misc = KeyPool('misc', 4)

        identf = A.alloc([128], F32)
        onesf = A.alloc([128], F32)
        onesm = A.alloc([128], F32)
        epsc = A.alloc([1], F32)
        silucc = A.alloc([16], F32)
        wr_sb = A.alloc([8, 16], F32)
        brt_bc = A.alloc([16], F32)
        hsel = A.alloc([2], F32)
        R_const = Res('const')
        dma('sp', identf, identd, [], [R_const])
        memset('pool', onesf, 1.0, [R_const])
        memset('pool', onesm, 1.0 / 512.0, [R_const])
        memset('pool', epsc, EPS, [R_const])
        dma('sp', silucc, ccT, [], [R_const])
        dma('sp', wr_sb.rearrange('p k e -> p (k e)'), w_router_r, [], [R_const])
        dma('sp', brt_bc, b_router.partition_broadcast(128), [], [R_const])
        dma('sp', hsel, halo_sel, [], [R_const])
        act(silucc, silucc, AF.Silu, [R_const], [R_const])

        modT = A.alloc([48, 2], F32); R_mod = Res('mod')
        modbc = A.alloc([4, D], F32); R_modbc = Res('modbc')
        lnbc = A.alloc([4, D], F32); R_lnbc = Res('lnbc')
        neglam = A.alloc([1], F32); R_lam = Res('lam')
        gsub = A.alloc([128], F32); R_gsub = Res('gsub')
        cw = A.alloc([4, 31], F32); cvv = A.alloc([3, 4], F32); R_cw = Res('cw')
        kcT = A.alloc([4, NCTX], BF16); R_kcT = Res('kcT')
        vcs = A.alloc([2, 4, 129], BF16); R_vcs = Res('vcs')
        qcT = A.alloc([4, NCTX], BF16); R_qcT = Res('qcT')

        blocks_lat = [(512 * b, 512) for b in range(8)]
        blk_ctx = (NLAT, NCTX)

        def x_in_ap(l, t0, n):
            if l == 0:
                if t0 < NLAT:
                    return x_own[t0:t0 + n, :]
                return ctxb[t0 - NLAT:t0 - NLAT + n, :]
            return xs2[t0:t0 + n, :]

        for l in range(n_layers):
            last = (l == 1)
            lam_init = 0.8 - 0.6 * math.exp(-0.3 * l)
            bounce = bounce_t[l]
            gath = gath_t[l]
            S.barrier()
            A.mark()
            A.mark()
            stage = [A.alloc([6 * D], F32) for _ in range(2)]
            R_stage = [Res() for _ in range(2)]
            modacc = A.alloc([96], F32); R_modacc = Res()
            badat = A.alloc([96], F32); R_badat = Res()
            lamt = A.alloc([256], F32); R_lamt = Res()
            lamp = A.alloc([128], F32)
            lams = A.alloc([4], F32)
            tmpb = A.alloc([128], F32); R_tmpb = Res()
            dma('sp', badat, b_adaT, [], [R_badat])
            dma('sp', lamt, lamv[l:l + 1, :].partition_broadcast(128), [], [R_lamt])
            dma('sp', gsub, sublng[l:l + 1, :].partition_broadcast(128), [], [R_gsub])
            dma('sp', lnbc.rearrange('p a d -> p (a d)'), lnv[l:l + 1, :].partition_broadcast(128), [], [R_lnbc])
            dma('sp', cw.rearrange('p c k -> p (c k)'), convw[:, l * 124:(l + 1) * 124], [], [R_cw])
            dma('sp', cvv.rearrange('p a c -> p (a c)'), convv[:, l * 12:(l + 1) * 12], [], [R_cw])
            for k in range(8):
                sb = k % 2
                dma('sp' if k % 2 == 0 else 'pool', stage[sb], w_ada[l, k * 128:(k + 1) * 128, :], [], [R_stage[sb]],
                    key=KP_ld.next())
                for ch in range(48):
                    mm(PS[0][:, 2 * ch:2 * ch + 2], stage[sb][:, ch * 128:(ch + 1) * 128], silucc[:, 2 * k:2 * k + 2],
                       True, True, [R_stage[sb], R_const], [RPS[0]])
                if k == 0:
                    cp('dve', modacc, PS[0][:, 0:96], [RPS[0]], [R_modacc])
                else:
                    tt('dve', modacc, modacc, PS[0][:, 0:96], ALU.add, [RPS[0], R_modacc], [R_modacc])
            macc3 = modacc.rearrange('p (c j) -> p c j', j=2)
            for j in range(2):
                tt('dve', modT[:, :, j], macc3[:, :, j], badat[:, l * 48:(l + 1) * 48], ALU.add,
                   [R_modacc, R_badat], [R_mod])
            for mi in (1, 4):
                ts('dve', modT[:, mi * 8:(mi + 1) * 8, :], modT[:, mi * 8:(mi + 1) * 8, :], 1.0, ALU.add, [R_mod], [R_mod])
            for bi, (mi, j) in enumerate([(2, 0), (2, 1), (5, 0), (5, 1)]):
                for kh in range(2):
                    for k4 in range(4):
                        k = kh * 4 + k4
                        ts('dve', tmpb, onesf, modT[:, mi * 8 + k, j:j + 1], ALU.mult, [R_mod, R_const, R_tmpb], [R_tmpb])
                        mm(PS[1][:, k4 * 128:(k4 + 1) * 128], tmpb, identf, True, True, [R_tmpb, R_const], [RPS[1]])
                    cp('dve', modbc[:, bi, kh * 512:(kh + 1) * 512], PS[1], [RPS[1]], [R_modbc])
            for i in range(2):
                tt('dve', lamp[:, 0:64], lamt[:, 128 * i:128 * i + 64], lamt[:, 128 * i + 64:128 * i + 128], ALU.mult,
                   [R_lamt, R_lam], [R_lam])
                S.op('dve', lambda e, i=i: e.reduce_sum(out=lams[:, i:i + 1], in_=lamp[:, 0:64], axis=AX.X),
                     [R_lam], [R_lam])
            act(lams[:, 0:2], lams[:, 0:2], AF.Exp, [R_lam], [R_lam])
            tt('dve', lams[:, 2:3], lams[:, 1:2], lams[:, 0:1], ALU.subtract, [R_lam], [R_lam])
            ts('dve', neglam, lams[:, 2:3], -lam_init, ALU.add, [R_lam], [R_lam])
            ts('dve', gsub, gsub, 1.0 - lam_init, ALU.mult, [R_gsub], [R_gsub])
            S.barrier()
            A.release()
            if stop == 'p0':
                S.emit(); print('sched stats', S.stats); return nc

            A.mark()
            R_qT = Res('qsc'); R_yT = Res('ysc'); R_ycT = Res('ycsc')
            A.mark()
            zt = A.alloc([4, 16], BF16); R_zt = Res()
            hxT = A.alloc([8, TOK], BF16)
            R_hx = [Res(f'hx{t}') for t in range(34)]
            xt_b = [A.alloc([D], F32) for _ in range(2)]; R_xt = [Res() for _ in range(2)]
            wst = [A.alloc([8, 256], F32) for _ in range(2)]; R_wst = [Res() for _ in range(2)]
            wbf = [A.alloc([8, 256], BF16) for _ in range(2)]; R_wbf = [Res() for _ in range(2)]
            rC = [A.alloc([512], F32) for _ in range(2)]; rS = [A.alloc([512], F32) for _ in range(2)]
            R_rope = [Res() for _ in range(2)]
            t1 = [A.alloc([512], F32) for _ in range(2)]; R_t1 = [Res() for _ in range(2)]
            t2 = [A.alloc([512], F32) for _ in range(2)]; R_t2 = [Res() for _ in range(2)]
            ot = [A.alloc([2, 512], BF16) for _ in range(3)]; R_ot = [Res() for _ in range(3)]
            vt = [A.alloc([2, 128], BF16) for _ in range(3)]; R_vt = [Res() for _ in range(3)]
            memset('pool', zt, 0.0, [R_zt])
            dma('pool', ycsc[:, :, 0:16], zt, [R_zt], [R_ycT])
            dma('pool', ycsc[:, :, NCTX + 16:NCTX + 32], zt, [R_zt], [R_ycT])
            for t in range(34):
                j = 0 if t < 32 else 1
                xb = t % 2
                dma('sp', xt_b[xb], x_in_ap(l, t * 128, 128), [], [R_xt[xb]], key=KP_ld.next())
                for kh in range(2):
                    pb = 2 + (2 * t + kh) % 2
                    for k4 in range(4):
                        k = kh * 4 + k4
                        tr(PS[pb][:, k4 * 128:(k4 + 1) * 128], xt_b[xb][:, k * 128:(k + 1) * 128], [R_xt[xb]], [RPS[pb]])
                    for k4 in range(4):
                        k = kh * 4 + k4
                        act(hxT[:, k, t * 128:(t + 1) * 128], PS[pb][:, k4 * 128:(k4 + 1) * 128], AF.Identity,
                            [RPS[pb], R_mod], [R_hx[t]], bias=modT[:, 0 * 8 + k, j:j + 1], scale=modT[:, 1 * 8 + k, j:j + 1])
            kreg = [bounce[h][:, :].rearrange('(p a) b -> p (a b)', p=128, a=8) for h in range(4)]
            ereg = bounce[8][:, :].rearrange('a b -> (a b)').rearrange(
                '(p c s t) -> p c s t', p=128, c=4, s=2, t=16)
            R_bounce = Res('bounce')
            R_gsc = Res('gsc')
            groups = ([('Q', h) for h in range(4)] + [('K', h) for h in range(4)] + [('V', g) for g in range(2)]
                      + [('U', c) for c in range(4)] + [('G', g) for g in range(8)])
            tok_blocks = blocks_lat + [blk_ctx]
            oti = 0
            vti = 0
            pbi = 0
            for gi, (kind, idx) in enumerate(groups):
                wb = gi % 2
                c0 = gi * 256
                dma('sp' if gi % 2 == 0 else 'pool', wst[wb],
                    w_in_r[l, :, c0:c0 + 256].rearrange('(k p) c -> p k c', p=128), [], [R_wst[wb]], key=KP_ld.next())
                cp('pool', wbf[wb], wst[wb], [R_wst[wb]], [R_wbf[wb]])
                for bi_, (t0, n) in enumerate(tok_blocks):
                    isctx = t0 >= NLAT
                    if isctx and last and kind in ('Q', 'U', 'G'):
                        continue
                    hx_res = [R_hx[t0 // 128 + i] for i in range(n // 128)]
                    if kind in ('Q', 'K', 'U', 'G'):
                        pa, pbk = 4 + (pbi % 2) * 2, 5 + (pbi % 2) * 2
                        pbi += 1
                        need_b = not (isctx and kind in ('Q', 'K'))
                        for half, pidx in ((0, pa), (1, pbk)):
                            if half == 1 and not need_b:
                                continue
                            for k in range(8):
                                mm(PS[pidx][:, 0:n], wbf[wb][:, k, half * 128:(half + 1) * 128], hxT[:, k, t0:t0 + n],
                                   k == 0, k == 7, [R_wbf[wb]] + hx_res, [RPS[pidx]])
                        if kind in ('Q', 'K'):
                            if isctx:
                                dst, rdst = (qcT, R_qcT) if kind == 'Q' else (kcT, R_kcT)
                                cp('dve', dst[:, idx, :], PS[pa][:, 0:n], [RPS[pa]], [rdst])
                            else:
                                rb = bi_ % 2
                                if gi == 0 or True:
                                    dma('sp', rC[rb], ropeC[:, t0:t0 + 512], [], [R_rope[rb]], key=KP_ld.next())
                                    dma('sp', rS[rb], ropeS[:, t0:t0 + 512], [], [R_rope[rb]], key=KP_ld.next())
                                tb = bi_ % 2
                                tt('dve', t1[tb], PS[pa], rC[rb], ALU.mult, [RPS[pa], R_rope[rb]], [R_t1[tb]])
                                tt('dve', t2[tb], PS[pbk], rS[rb], ALU.mult, [RPS[pbk], R_rope[rb]], [R_t2[tb]])
                                o = oti % 3
                                oti += 1
                                tt('pool', ot[o][:, 0, :], t1[tb], t2[tb], ALU.add, [R_t1[tb], R_t2[tb]], [R_ot[o]])
                                if kind == 'Q':
                                    dma('pool', qsc[idx, :, t0:t0 + 512], ot[o][:, 0, :], [R_ot[o]], [R_qT],
                                        key=KP_st.next())
                                else:
                                    dma('pool', kreg[idx][:, t0:t0 + 512], ot[o][:, 0, :], [R_ot[o]], [R_bounce],
                                        key=KP_st.next())
                        elif kind == 'U':
                            tb = bi_ % 2
                            act(t1[tb][:, 0:n], PS[pbk][:, 0:n], AF.Sigmoid, [RPS[pbk]], [R_t1[tb]])
                            o = oti % 3
                            oti += 1
                            tt('dve', ot[o][:, 0, 0:n], PS[pa][:, 0:n], t1[tb][:, 0:n], ALU.mult,
                               [RPS[pa], R_t1[tb]], [R_ot[o]])
                            if isctx:
                                dma('pool', ycsc[:, idx, 16:16 + n], ot[o][:, 0, 0:n], [R_ot[o]], [R_ycT], key=KP_st.next())
                            else:
                                dma('pool', ysc[:, idx, 16 + t0:16 + t0 + n], ot[o][:, 0, 0:n], [R_ot[o]], [R_yT],
                                    key=KP_st.next())
                        else:
                            o = oti % 3
                            oti += 1
                            act(ot[o][:, 0, 0:n], PS[pa][:, 0:n], AF.Sigmoid, [RPS[pa]], [R_ot[o]])
                            act(ot[o][:, 1, 0:n], PS[pbk][:, 0:n], AF.Sigmoid, [RPS[pbk]], [R_ot[o]])
                            dma('pool', gsc[:, 2 * idx:2 * idx + 2, t0:t0 + n], ot[o][:, :, 0:n], [R_ot[o]], [R_gsc],
                                key=KP_st.next())
                    else:
                        for tl_ in range(n // 128):
                            tk0 = t0 + tl_ * 128
                            pv = 4 + (pbi % 4)
                            pbi += 1
                            for k in range(8):
                                mm(PS[pv][:, 0:256], hxT[:, k, tk0:tk0 + 128], wbf[wb][:, k, :], k == 0, k == 7,
                                   [R_wbf[wb], R_hx[tk0 // 128]], [RPS[pv]])
                            if isctx:
                                cp('dve', vcs[:, tl_, 2 * idx:2 * idx + 2, 0:128],
                                   PS[pv][:, 0:256].rearrange('p (h e) -> p h e', h=2), [RPS[pv]], [R_vcs])
                            else:
                                v = vti % 3
                                vti += 1
                                cp('dve', vt[v][:, :, 0:128], PS[pv][:, 0:256].rearrange('p (h e) -> p h e', h=2),
                                   [RPS[pv]], [R_vt[v]])
                                dma('pool', bounce[4 + tk0 // 1024][tk0 % 1024:tk0 % 1024 + 128, idx * 256:(idx + 1) * 256],
                                    vt[v].rearrange('p h e -> p (h e)'), [R_vt[v]], [R_bounce], key=KP_st.next())
            memset('pool', vcs[:, :, :, 128:129], 1.0, [R_vcs])
            dma('pool', ereg[:, :, 0, :], ysc[:, :, 16:32], [R_yT], [R_bounce], key=KP_st.next())
            dma('pool', ereg[:, :, 1, :], ysc[:, :, NLAT:NLAT + 16], [R_yT], [R_bounce], key=KP_st.next())
            if stop == 'A':
                S.emit(); print('sched stats', S.stats); return nc
            R_gc = [Res(f'gath{c}') for c in range(9)]
            for c in range(9):
                cckey = S.dma_key(f'cc{l}_{c}', cc=True)
                S.op('pool', lambda e, b_=bounce[c], g_=gath[c]: e.collective_compute(
                    "AllGather", ALU.bypass, replica_groups=[[0, 1], [2, 3], [4, 5], [6, 7]],
                    ins=[b_.ap().opt()], outs=[g_.ap().opt()]), [R_bounce], [R_gc[c]], dma=cckey)
            S.barrier()
            A.release()
            A.mark()
            egs = [gath[8][r * 32:(r + 1) * 32, :].rearrange('a b -> (a b)').rearrange(
                '(p c s t) -> p c s t', p=128, c=4, s=2, t=16) for r in range(2)]
            dma('sp', ysc[:, :, 0:16], egs[0][:, :, 1, :], [R_gc[8]], [R_yT])
            dma('sp', ysc[:, :, NLAT + 16:NLAT + 32], egs[1][:, :, 0, :], [R_gc[8]], [R_yT])

            if stop == 'X':
                S.emit(); print('sched stats', S.stats); return nc
            R_anT = Res('ansc')
            A.mark()
            anst = [A.alloc([512], BF16) for _ in range(2)]; R_anst = [Res() for _ in range(2)]
            qblk = [A.alloc([512], BF16) for _ in range(2)]; R_qblk = [Res() for _ in range(2)]
            qbi = 0
            kT = [A.alloc([2 * NLAT], BF16) for _ in range(2)]; R_kT = [Res() for _ in range(2)]
            vh = [A.alloc([64, 129], BF16) for _ in range(2)]; R_vh = [Res() for _ in range(2)]
            pT = [A.alloc([1024], BF16) for _ in range(3)]; R_pT = [Res() for _ in range(3)]
            for i in range(2):
                memset('pool', vh[i][:, :, 128:129], 1.0, [R_vh[i]])
            nrm = A.alloc([8], F32); R_nrm = Res()
            tO = A.alloc([128], F32); oO = A.alloc([128], F32); onf = A.alloc([128], F32); R_o = Res()
            junk = A.alloc([128], F32)
            pti = 0
            sci = 0
            pst = [A.alloc([4, 512], F32) for _ in range(3)]; R_pst = [Res() for _ in range(3)]
            pbf = [A.alloc([4, 512], BF16) for _ in range(2)]; R_pbf = [Res() for _ in range(2)]
            R_wc3 = Res('wc3')
            R_wc = [Res(f'wc{e_}') for e_ in range(NE)]
            wc_3v = wc_3.rearrange('p (a d) -> p a d', a=16)
            pieces = []
            for hh in range(2):
                pieces.append((w_attn_o[l, :, hh * 512:(hh + 1) * 512].rearrange('(k p) c -> p k c', p=128),
                               wc_3v[:, 0:4, hh * 512:(hh + 1) * 512], R_wc3))
                pieces.append((w_conv_o[l, :, hh * 512:(hh + 1) * 512].rearrange('(k p) c -> p k c', p=128),
                               wc_3v[:, 4:8, hh * 512:(hh + 1) * 512], R_wc3))
                for kh in range(2):
                    pieces.append((w_out[l, kh * 512:(kh + 1) * 512, hh * 512:(hh + 1) * 512].rearrange('(k p) c -> p k c', p=128),
                                   wc_3v[:, 8 + kh * 4:8 + (kh + 1) * 4, hh * 512:(hh + 1) * 512], R_wc3))
            for ex in range(NE):
                wgv = wc_g[ex].rearrange('p (k c) -> p k c', k=8)
                wuv = wc_u[ex].rearrange('p (k c) -> p k c', k=8)
                wdv = wc_d[ex].rearrange('p (k c) -> p k c', k=4)
                for kh in range(2):
                    pieces.append((w_e_gate[l, ex, kh * 512:(kh + 1) * 512, :].rearrange('(k p) c -> p k c', p=128),
                                   wgv[:, kh * 4:(kh + 1) * 4, :], R_wc[ex]))
                    pieces.append((w_e_up[l, ex, kh * 512:(kh + 1) * 512, :].rearrange('(k p) c -> p k c', p=128),
                                   wuv[:, kh * 4:(kh + 1) * 4, :], R_wc[ex]))
                for hh in range(2):
                    pieces.append((w_e_down[l, ex, :, hh * 512:(hh + 1) * 512].rearrange('(k p) c -> p k c', p=128),
                                   wdv[:, :, hh * 512:(hh + 1) * 512], R_wc[ex]))
            pci = [0]

            def emit_pieces(cnt):
                for _ in range(cnt):
                    if pci[0] >= len(pieces):
                        return
                    src, dst, rdst = pieces[pci[0]]
                    i3 = pci[0] % 3
                    i2 = pci[0] % 2
                    pci[0] += 1
                    dma('sp', pst[i3], src, [], [R_pst[i3]], key=KP_ld.next())
                    cp('pool' if pci[0] % 3 else 'dve', pbf[i2], pst[i3], [R_pst[i3]], [R_pbf[i2]])
                    dma('pool', dst, pbf[i2], [R_pbf[i2]], [rdst], key=KP_st.next())
            for h in range(4):
                hb = h % 2
                for r in range(2):
                    kg = gath[h][r * 1024:(r + 1) * 1024, :].rearrange('(p a) b -> p (a b)', p=128, a=8)
                    dma('sp', kT[hb][:, r * NLAT:(r + 1) * NLAT], kg, [R_gc[h]], [R_kT[hb]], key=KP_ld.next())
                    for q4 in range(4):
                        vg = gath[4 + q4][r * 1024:(r + 1) * 1024, h * 128:(h + 1) * 128].rearrange('(t p) e -> p t e', p=128)
                        dma('pool', vh[hb][:, r * 32 + q4 * 8:r * 32 + q4 * 8 + 8, 0:128],
                            vg, [R_gc[4 + q4]], [R_vh[hb]], key=KP_ld.next())
                qblocks = [(t0, n, False) for (t0, n) in blocks_lat]
                if not last:
                    qblocks.append((NLAT, NCTX, True))
                for (t0, n, isctx) in qblocks:
                    nqt = n // 128
                    qb_ = qbi % 2
                    qbi += 1
                    if not isctx:
                        dma('sp', qblk[qb_], qsc[h, :, t0:t0 + n], [R_qT], [R_qblk[qb_]], key=KP_ld.next())
                    ktiles = [('c', i) for i in range(2)]
                    if not isctx:
                        ktiles += [('l', i) for i in range(64)]
                    def emit_qk(kti):
                        kk, ki = ktiles[kti]
                        sb0 = 4 + (kti % 2) * 2
                        for m in range(2):
                            if kk == 'c':
                                lh = kcT[64 * m:64 * m + 64, h, ki * 128:(ki + 1) * 128]
                                rr = [R_kcT]
                            else:
                                lh = kT[hb][64 * m:64 * m + 64, ki * 128:(ki + 1) * 128]
                                rr = [R_kT[hb]]
                            if isctx:
                                rh = qcT[64 * m:64 * m + 64, h, :]
                                rr = rr + [R_qcT]
                            else:
                                rh = qblk[qb_][64 * m:64 * m + 64, 0:n]
                                rr = rr + [R_qblk[qb_]]
                            mm(PS[sb0 + m][:, 0:n], lh, rh, True, True, rr, [RPS[sb0 + m]], tp=(64 * m, 0))
                    emit_qk(0)
                    for kti, (kk, ki) in enumerate(ktiles):
                        sb0 = 4 + (kti % 2) * 2
                        if kti + 1 < len(ktiles):
                            emit_qk(kti + 1)
                        p = pti % 3
                        pti += 1
                        act(pT[p].rearrange('p (m q) -> p m q', m=2)[:, :, 0:n],
                            SC[(sb0 - 4) // 2].rearrange('p (m q) -> p m q', m=2)[:, :, 0:n], AF.Exp,
                            [RPS[sb0], RPS[sb0 + 1]], [R_pT[p]], scale=0.125)
                        for qt in range(nqt):
                            for m in range(2):
                                if kk == 'c':
                                    vv = vcs[:, ki, h, :]
                                    rv = [R_vcs]
                                else:
                                    vv = vh[hb][:, ki, :]
                                    rv = [R_vh[hb]]
                                mm(PS[qt][:, m * 129:(m + 1) * 129], pT[p][:, m * 512 + qt * 128:m * 512 + qt * 128 + 128], vv,
                                   (kti == 0 and m == 0), kti == len(ktiles) - 1, [R_pT[p]] + rv, [RPS[qt]], skip=True)
                    for qt in range(nqt):
                        acc = PS[qt]
                        recip(nrm[:, 0:1], acc[:, 128:129], [RPS[qt], R_nrm], [R_nrm])
                        recip(nrm[:, 1:2], acc[:, 257:258], [RPS[qt], R_nrm], [R_nrm])
                        tt('dve', nrm[:, 2:3], nrm[:, 1:2], neglam, ALU.mult, [R_nrm, R_lam], [R_nrm])
                        ts('dve', tO, acc[:, 129:257], nrm[:, 2:3], ALU.mult, [RPS[qt], R_nrm, R_o], [R_o])
                        stt('dve', oO, acc[:, 0:128], nrm[:, 0:1], tO, ALU.mult, ALU.add, [RPS[qt], R_nrm, R_o], [R_o])
                        memset('dve', nrm[:, 3:4], 0.0, [R_nrm])
                        act(junk, oO, AF.Square, [R_o], [R_o, R_nrm], accum=nrm[:, 3:4])
                        act(nrm[:, 4:5], nrm[:, 3:4], AF.Sqrt, [R_nrm, R_const], [R_nrm], bias=epsc, scale=1.0 / 128.0)
                        recip(nrm[:, 5:6], nrm[:, 4:5], [R_nrm], [R_nrm])
                        stt('dve', onf, oO, nrm[:, 5:6], gsub, ALU.mult, ALU.mult, [R_o, R_nrm, R_gsub], [R_o])
                        tr(PS[4][:, qt * 128:(qt + 1) * 128], onf, [R_o], [RPS[4]])
                        cp('dve', anst[qb_][:, qt * 128:(qt + 1) * 128], PS[4][:, qt * 128:(qt + 1) * 128],
                           [RPS[4]], [R_anst[qb_]])
                    dma('sp', ansc[:, h, t0:t0 + n], anst[qb_][:, 0:n], [R_anst[qb_]], [R_anT], key=KP_st.next())
                    emit_pieces(4)
            emit_pieces(len(pieces))
            S.barrier()
            A.release()

            if stop == 'B':
                S.emit(); print('sched stats', S.stats); return nc
            A.mark()
            wst2 = [A.alloc([4, 512], F32) for _ in range(3)]; R_wst2 = [Res() for _ in range(3)]
            wsi = [0]

            def load_cast(dst_ap, src_ap, rdst):
                i = wsi[0] % 3
                wsi[0] += 1
                dma('sp', wst2[i], src_ap, [], [R_wst2[i]], key=KP_ld.next())
                if wsi[0] % 3 == 0:
                    cp('act', dst_ap, wst2[i], [R_wst2[i]], [rdst])
                else:
                    cp('pool', dst_ap, wst2[i], [R_wst2[i]], [rdst])
            SBT = 512
            NST = SBT // 128
            hx2T = A.alloc([8, SBT], BF16); R_hx2 = [Res() for _ in range(NST)]
            macc = A.alloc([NST, D], F32); R_macc = [Res() for _ in range(NST)]
            gts = A.alloc([NST, 16], F32); R_gts = [Res() for _ in range(NST)]
            xin = [A.alloc([D], F32) for _ in range(2)]; R_xin = [Res() for _ in range(2)]
            yb = [A.alloc([D], F32) for _ in range(2)]; R_yb = [Res() for _ in range(2)]
            stat = A.alloc([12], F32); mv = A.alloc([4], F32); R_mv = Res()
            hx2f = A.alloc([8, 128], F32); R_hx2f = Res()
            rt = A.alloc([16 * 8], F32); R_rt = Res()
            xi_i = [0]

            def layer_norm_tile(yt, ry, gi, dst_dram, rdst):
                for c2 in range(2):
                    S.op('dve', lambda e, c2=c2: e.bn_stats(out=stat[:, c2 * 6:(c2 + 1) * 6], in_=yt[:, c2 * 512:(c2 + 1) * 512]),
                         [ry], [R_mv])
                S.op('dve', lambda e: e.bn_aggr(out=mv[:, 0:2], in_=stat), [R_mv], [R_mv])
                act(mv[:, 2:3], mv[:, 1:2], AF.Sqrt, [R_mv, R_const], [R_mv], bias=epsc, scale=1.0)
                recip(mv[:, 3:4], mv[:, 2:3], [R_mv], [R_mv])
                ts('dve', yt, yt, mv[:, 0:1], ALU.subtract, [ry, R_mv], [ry], s2=mv[:, 3:4], op1=ALU.mult)
                tt('pool', yt, yt, lnbc[:, gi, :], ALU.mult, [ry, R_lnbc], [ry])
                tt('pool', yt, yt, lnbc[:, gi + 1, :], ALU.add, [ry, R_lnbc], [ry])
                dma('sp', dst_dram, yt, [ry], [rdst], key=KP_st.next())

            R_xs1 = Res('xs1')
            R_out = Res('out')
            all_blocks = list(blocks_lat) + ([blk_ctx] if not last else [])
            for (t0, n) in all_blocks:
                isctx = t0 >= NLAT
                j = 1 if isctx else 0
                A.mark()
                w3all = A.alloc([16, D], BF16)
                wao = w3all[:, 0:4, :]; wco = w3all[:, 4:8, :]; wo = w3all[:, 8:16, :]
                R_w3 = Res('w3')
                first_blk = False
                if not first_blk:
                    dma('sp', w3all.rearrange('p a d -> p (a d)'), wc_3, [R_wc3], [R_w3], key=KP_ld.next())
                for hh in (range(2) if first_blk else []):
                    load_cast(wao[:, :, hh * 512:(hh + 1) * 512],
                              w_attn_o[l, :, hh * 512:(hh + 1) * 512].rearrange('(k p) c -> p k c', p=128), R_w3)
                    load_cast(wco[:, :, hh * 512:(hh + 1) * 512],
                              w_conv_o[l, :, hh * 512:(hh + 1) * 512].rearrange('(k p) c -> p k c', p=128), R_w3)
                    for kh in range(2):
                        load_cast(wo[:, kh * 4:(kh + 1) * 4, hh * 512:(hh + 1) * 512],
                                  w_out[l, kh * 512:(kh + 1) * 512, hh * 512:(hh + 1) * 512].rearrange('(k p) c -> p k c', p=128),
                                  R_w3)
                if first_blk:
                    dma('pool', wc_3, w3all.rearrange('p a d -> p (a d)'), [R_w3], [R_wc3], key=KP_st.next())
                z = A.alloc([4, 512], F32); R_z = [Res() for _ in range(4)]
                zq = A.alloc([512], F32); R_zq = Res()
                mean = A.alloc([512], F32); rstd = A.alloc([512], F32); R_st = Res()
                snT = A.alloc([4, 512], BF16); R_snT = Res()
                gtl = [A.alloc([2, 512], BF16) for _ in range(2)]; R_gtl = [Res() for _ in range(2)]
                GT = A.alloc([8, 512], BF16); R_GT = Res()
                tg1 = A.alloc([512], F32); tg2 = A.alloc([512], F32); R_tg = Res()
                yblk = A.alloc([4, 512 + 32], BF16); R_yblk = Res()
                anblk = A.alloc([4, 512], BF16); R_anblk = Res()
                if isctx:
                    dma('sp', yblk[:, :, 0:n + 32], ycsc[:, :, 0:n + 32], [R_ycT], [R_yblk], key=KP_ld.next())
                else:
                    dma('sp', yblk[:, :, 0:n + 32], ysc[:, :, t0:t0 + n + 32], [R_yT], [R_yblk], key=KP_ld.next())
                    if t0 == 0:
                        ts('dve', yblk[:, :, 0:16], yblk[:, :, 0:16], hsel[:, 0:1], ALU.mult, [R_yblk, R_const], [R_yblk])
                    if t0 + n == NLAT:
                        ts('dve', yblk[:, :, n + 16:n + 32], yblk[:, :, n + 16:n + 32], hsel[:, 1:2], ALU.mult,
                           [R_yblk, R_const], [R_yblk])
                dma('sp', anblk[:, :, 0:n], ansc[:, :, t0:t0 + n], [R_anT], [R_anblk], key=KP_ld.next())
                for c in range(4):
                    ts('dve', z[:, c, 0:n], yblk[:, c, 1:1 + n], cw[:, c, 0:1], ALU.mult,
                       [R_yblk, R_cw], [R_z[c]], s2=cvv[:, 0, c:c + 1], op1=ALU.add)
                    for k in range(1, 31):
                        stt('dve', z[:, c, 0:n], yblk[:, c, k + 1:k + 1 + n], cw[:, c, k:k + 1], z[:, c, 0:n],
                            ALU.mult, ALU.add, [R_yblk, R_cw], [R_z[c]])
                for c in range(4):
                    mm(PS[0][:, 0:n], onesm, z[:, c, 0:n], c == 0, c == 3, [R_const, R_z[c]], [RPS[0]])
                for c in range(4):
                    act(zq[:, 0:n], z[:, c, 0:n], AF.Square, [R_z[c]], [R_zq])
                    mm(PS[1][:, 0:n], onesm, zq[:, 0:n], c == 0, c == 3, [R_const, R_zq], [RPS[1]])
                cp('dve', mean[:, 0:n], PS[0][:, 0:n], [RPS[0]], [R_st])
                tt('dve', rstd[:, 0:n], mean[:, 0:n], mean[:, 0:n], ALU.mult, [R_st], [R_st])
                tt('dve', rstd[:, 0:n], PS[1][:, 0:n], rstd[:, 0:n], ALU.subtract, [RPS[1], R_st], [R_st])
                act(rstd[:, 0:n], rstd[:, 0:n], AF.Sqrt, [R_st, R_const], [R_st], bias=epsc, scale=1.0)
                recip(rstd[:, 0:n], rstd[:, 0:n], [R_st], [R_st])
                for c in range(4):
                    tt('dve', z[:, c, 0:n], z[:, c, 0:n], mean[:, 0:n], ALU.subtract, [R_st, R_z[c]], [R_z[c]])
                    tt('dve', z[:, c, 0:n], z[:, c, 0:n], rstd[:, 0:n], ALU.mult, [R_st, R_z[c]], [R_z[c]])
                    act(snT[:, c, 0:n], z[:, c, 0:n], AF.Silu, [R_z[c], R_cw], [R_snT],
                        bias=cvv[:, 2, c:c + 1], scale=cvv[:, 1, c:c + 1])
                for oc in range(8):
                    pa2 = 2 + (oc % 2) * 2
                    g_i = oc % 2
                    dma('sp', gtl[g_i][:, 0, 0:n], gsc[:, oc, t0:t0 + n], [R_gsc], [R_gtl[g_i]], key=KP_ld.next())
                    dma('sp', gtl[g_i][:, 1, 0:n], gsc[:, 8 + oc, t0:t0 + n], [R_gsc], [R_gtl[g_i]], key=KP_ld.next())
                    for hh in range(4):
                        mm(PS[pa2][:, 0:n], wao[:, hh, oc * 128:(oc + 1) * 128], anblk[:, hh, 0:n], hh == 0, hh == 3,
                           [R_w3, R_anblk], [RPS[pa2]])
                    for c in range(4):
                        mm(PS[pa2 + 1][:, 0:n], wco[:, c, oc * 128:(oc + 1) * 128], snT[:, c, 0:n], c == 0, c == 3,
                           [R_w3, R_snT], [RPS[pa2 + 1]])
                    tt('dve', tg1[:, 0:n], PS[pa2][:, 0:n], gtl[g_i][:, 0, 0:n], ALU.mult,
                       [RPS[pa2], R_gtl[g_i], R_tg], [R_tg])
                    tt('dve', tg2[:, 0:n], PS[pa2 + 1][:, 0:n], gtl[g_i][:, 1, 0:n], ALU.mult,
                       [RPS[pa2 + 1], R_gtl[g_i], R_tg], [R_tg])
                    tt('pool', GT[:, oc, 0:n], tg1[:, 0:n], tg2[:, 0:n], ALU.add, [R_tg], [R_GT])
                for tl_ in range(n // 128):
                    tk0 = t0 + tl_ * 128
                    sti = tl_
                    xi = xi_i[0] % 2
                    xi_i[0] += 1
                    dma('sp', xin[xi], x_in_ap(l, tk0, 128), [], [R_xin[xi]], key=KP_ld.next())
                    for hf in range(2):
                        pm = 6 + hf
                        for oc in range(8):
                            mm(PS[pm], GT[:, oc, tl_ * 128:(tl_ + 1) * 128], wo[:, oc, hf * 512:(hf + 1) * 512],
                               oc == 0, oc == 7, [R_GT, R_w3], [RPS[pm]])
                        tt('dve', yb[xi][:, hf * 512:(hf + 1) * 512], PS[pm], modbc[:, 0 + j, hf * 512:(hf + 1) * 512],
                           ALU.mult, [RPS[pm], R_modbc], [R_yb[xi]])
                    stt('dve', yb[xi], xin[xi], ALPHA, yb[xi], ALU.mult, ALU.add, [R_xin[xi], R_yb[xi]], [R_yb[xi]])
                    layer_norm_tile(yb[xi], R_yb[xi], 0, xs1[tk0:tk0 + 128, :], R_xs1)
                    for kh in range(2):
                        pb = 0 + kh
                        for k4 in range(4):
                            k = kh * 4 + k4
                            tr(PS[pb][:, k4 * 128:(k4 + 1) * 128], yb[xi][:, k * 128:(k + 1) * 128], [R_yb[xi]], [RPS[pb]])
                        for k4 in range(4):
                            k = kh * 4 + k4
                            act(hx2f[:, k, :], PS[pb][:, k4 * 128:(k4 + 1) * 128], AF.Identity, [RPS[pb], R_mod], [R_hx2f],
                                bias=modT[:, 3 * 8 + k, j:j + 1], scale=modT[:, 4 * 8 + k, j:j + 1])
                    cp('pool', hx2T[:, :, sti * 128:(sti + 1) * 128], hx2f, [R_hx2f], [R_hx2[sti]])
                    for k in range(8):
                        mm(PS[0][:, 0:16], hx2f[:, k, :], wr_sb[:, k, :], k == 0, k == 7, [R_hx2f, R_const], [RPS[0]])
                    lg = rt[:, 0:16]; pr = rt[:, 16:32]; pm6 = rt[:, 32:56]; gsc4 = rt[:, 56:60]
                    sc1 = rt[:, 60:61]; sc2 = rt[:, 61:62]; sc3 = rt[:, 62:63]; sc4 = rt[:, 63:64]
                    gm = rt[:, 64:68]; mk = rt[:, 68:84]; mk2 = rt[:, 84:100]; ok16 = rt[:, 100:116]
                    RR = [R_rt]
                    tt('dve', lg, PS[0][:, 0:16], brt_bc, ALU.add, [RPS[0], R_const, R_rt], RR)
                    S.op('dve', lambda e, lg=lg, sc1=sc1: e.reduce_max(out=sc1, in_=lg, axis=AX.X), RR, RR)
                    ts('dve', sc2, sc1, -1.0, ALU.mult, RR, RR)
                    memset('dve', sc3, 0.0, RR)
                    act(pr, lg, AF.Exp, RR, RR, bias=sc2, scale=1.0, accum=sc3)
                    recip(sc4, sc3, RR, RR)
                    ts('dve', pr, pr, sc4, ALU.mult, RR, RR)
                    pr4 = pr.rearrange('p (g e) -> p g e', e=4)
                    pm64 = pm6.rearrange('p (g s) -> p g s', s=6)
                    for si, (a_, b_) in enumerate([(0, 1), (0, 2), (0, 3), (1, 2), (1, 3), (2, 3)]):
                        tt('dve', pm64[:, :, si], pr4[:, :, a_], pr4[:, :, b_], ALU.add, RR, RR)
                    S.op('dve', lambda e, pm64=pm64, gsc4=gsc4: e.tensor_reduce(out=gsc4, in_=pm64, axis=AX.X, op=ALU.max),
                         RR, RR)
                    S.op('dve', lambda e, gsc4=gsc4, sc1=sc1: e.reduce_max(out=sc1, in_=gsc4, axis=AX.X), RR, RR)
                    ts('dve', gm, gsc4, sc1, ALU.is_ge, RR, RR)
                    ok3 = ok16.rearrange('p (g e) -> p g e', e=4)
                    for e4 in range(4):
                        cp('dve', ok3[:, :, e4], gm, RR, RR)
                    ts('dve', mk, pr, 1.0, ALU.add, RR, RR)
                    tt('dve', mk, mk, ok16, ALU.mult, RR, RR)
                    ts('dve', mk, mk, -1.0, ALU.add, RR, RR)
                    S.op('dve', lambda e, mk=mk, sc1=sc1: e.reduce_max(out=sc1, in_=mk, axis=AX.X), RR, RR)
                    ts('dve', mk2, mk, sc1, ALU.is_ge, RR, RR)
                    stt('dve', mk2, mk2, -2.0, mk, ALU.mult, ALU.add, RR, RR)
                    S.op('dve', lambda e, mk2=mk2, sc2=sc2: e.reduce_max(out=sc2, in_=mk2, axis=AX.X), RR, RR)
                    tt('dve', sc3, sc1, sc2, ALU.add, RR, RR)
                    recip(sc4, sc3, RR, RR)
                    ts('dve', mk2, mk, sc2, ALU.is_ge, RR, RR)
                    tt('dve', mk2, mk2, pr, ALU.mult, RR, RR)
                    ts('dve', gts[:, sti, :], mk2, sc4, ALU.mult, RR + [R_gts[sti]], [R_gts[sti]])
                S.barrier()
                A.release()
                if stop == 'C':
                    S.emit(); print('sched stats', S.stats); return nc
                A.mark()
                weg = [A.alloc([8, 512], BF16) for _ in range(2)]; weu = [A.alloc([8, 512], BF16) for _ in range(2)]
                wed = [A.alloc([4, D], BF16) for _ in range(2)]; R_we = [Res() for _ in range(2)]
                sg = [A.alloc([512], F32) for _ in range(2)]; R_sg = [Res() for _ in range(2)]
                hT = [A.alloc([4, 512], BF16) for _ in range(2)]; R_hT = [Res() for _ in range(2)]
                hres = [R_hx2[i] for i in range(n // 128)]
                for ex in range(NE):
                    wb = ex % 2
                    if not first_blk:
                        dma('sp', weg[wb].rearrange('p a d -> p (a d)'), wc_g[ex], [R_wc[ex]], [R_we[wb]], key=KP_ld.next())
                        dma('sp', weu[wb].rearrange('p a d -> p (a d)'), wc_u[ex], [R_wc[ex]], [R_we[wb]], key=KP_ld.next())
                        dma('sp', wed[wb].rearrange('p a d -> p (a d)'), wc_d[ex], [R_wc[ex]], [R_we[wb]], key=KP_ld.next())
                    for hh in (range(2) if first_blk else []):
                        load_cast(weg[wb][:, hh * 4:(hh + 1) * 4, :],
                                  w_e_gate[l, ex, hh * 512:(hh + 1) * 512, :].rearrange('(k p) c -> p k c', p=128), R_we[wb])
                    for hh in (range(2) if first_blk else []):
                        load_cast(weu[wb][:, hh * 4:(hh + 1) * 4, :],
                                  w_e_up[l, ex, hh * 512:(hh + 1) * 512, :].rearrange('(k p) c -> p k c', p=128), R_we[wb])
                    for hh in (range(2) if first_blk else []):
                        load_cast(wed[wb][:, :, hh * 512:(hh + 1) * 512],
                                  w_e_down[l, ex, :, hh * 512:(hh + 1) * 512].rearrange('(k p) c -> p k c', p=128), R_we[wb])
                    if first_blk:
                        dma('pool', wc_g[ex], weg[wb].rearrange('p a d -> p (a d)'), [R_we[wb]], [R_wc[ex]], key=KP_st.next())
                        dma('pool', wc_u[ex], weu[wb].rearrange('p a d -> p (a d)'), [R_we[wb]], [R_wc[ex]], key=KP_st.next())
                        dma('pool', wc_d[ex], wed[wb].rearrange('p a d -> p (a d)'), [R_we[wb]], [R_wc[ex]], key=KP_st.next())
                    hb_ = ex % 2
                    for dc in range(4):
                        pg = 2 + (dc % 2) * 2
                        for k in range(8):
                            mm(PS[pg][:, 0:n], weg[wb][:, k, dc * 128:(dc + 1) * 128], hx2T[:, k, 0:n], k == 0, k == 7,
                               [R_we[wb]] + hres, [RPS[pg]])
                        for k in range(8):
                            mm(PS[pg + 1][:, 0:n], weu[wb][:, k, dc * 128:(dc + 1) * 128], hx2T[:, k, 0:n], k == 0, k == 7,
                               [R_we[wb]] + hres, [RPS[pg + 1]])
                        si = dc % 2
                        act(sg[si][:, 0:n], PS[pg][:, 0:n], AF.Silu, [RPS[pg]], [R_sg[si]])
                        tt('dve', hT[hb_][:, dc, 0:n], sg[si][:, 0:n], PS[pg + 1][:, 0:n], ALU.mult,
                           [R_sg[si], RPS[pg + 1]], [R_hT[hb_]])
                    for tl_ in range(n // 128):
                        sti = tl_
                        for hf in range(2):
                            pm = 6 + hf
                            for dc in range(4):
                                mm(PS[pm], hT[hb_][:, dc, tl_ * 128:(tl_ + 1) * 128], wed[wb][:, dc, hf * 512:(hf + 1) * 512],
                                   dc == 0, dc == 3, [R_hT[hb_], R_we[wb]], [RPS[pm]])
                            if ex == 0:
                                ts('dve', macc[:, sti, hf * 512:(hf + 1) * 512], PS[pm], gts[:, sti, ex:ex + 1], ALU.mult,
                                   [RPS[pm], R_gts[sti]], [R_macc[sti]])
                            else:
                                stt('dve', macc[:, sti, hf * 512:(hf + 1) * 512], PS[pm], gts[:, sti, ex:ex + 1],
                                    macc[:, sti, hf * 512:(hf + 1) * 512], ALU.mult, ALU.add,
                                    [RPS[pm], R_gts[sti]], [R_macc[sti]])
                for tl_ in range(n // 128):
                    tk0 = t0 + tl_ * 128
                    sti = tl_
                    xi = xi_i[0] % 2
                    xi_i[0] += 1
                    dma('sp', xin[xi], xs1[tk0:tk0 + 128, :], [R_xs1], [R_xin[xi]], key=KP_ld.next())
                    tt('dve', yb[xi], macc[:, sti, :], modbc[:, 2 + j, :], ALU.mult, [R_macc[sti], R_modbc], [R_yb[xi]])
                    stt('dve', yb[xi], xin[xi], ALPHA, yb[xi], ALU.mult, ALU.add, [R_xin[xi], R_yb[xi]], [R_yb[xi]])
                    if last:
                        layer_norm_tile(yb[xi], R_yb[xi], 2, out[tk0:tk0 + 128, :], R_out)
                    else:
                        layer_norm_tile(yb[xi], R_yb[xi], 2, xs2[tk0:tk0 + 128, :], R_out)
                S.barrier()
                A.release()
                if stop == 'D':
                    S.emit(); print('sched stats', S.stats); return nc
            S.barrier()
            A.release()
            A.release()
            A.release()
            A.release()
        S.emit()
    print('sched stats', S.stats, 'arena peak', A.peak)
    return nc


def _perm_cols():
    cols = []
    for base in (0, 512):
        for h in range(4):
            prim, sw = [], []
            for m in range(2):
                o = base + h * 128 + m * 64
                p_ = [o + 2 * i for i in range(32)] + [o + 2 * i + 1 for i in range(32)]
                s_ = [o + 2 * i + 1 for i in range(32)] + [o + 2 * i for i in range(32)]
                prim += p_
                sw += s_
            cols += prim + sw
    cols += list(range(1024, 1536))
    for c in range(4):
        cols += list(range(1536 + c * 128, 1536 + (c + 1) * 128))
        cols += list(range(2048 + c * 128, 2048 + (c + 1) * 128))
    cols += list(range(2560, 4608))
    return np.array(cols, dtype=np.int64)


def _rope_tables(half):
    t = np.arange(half * NLAT, (half + 1) * NLAT)
    row = (t // 64).astype(np.float32)
    col = (t % 64).astype(np.float32)
    inv_freq = (np.float32(10000.0) ** (-np.arange(0, 32, 2, dtype=np.float32) / np.float32(32))).astype(np.float32)
    ang = np.concatenate([row[:, None] * inv_freq, col[:, None] * inv_freq], -1).astype(np.float32)
    c = np.cos(ang).astype(np.float32).T
    s = np.sin(ang).astype(np.float32).T
    C = np.concatenate([c, c, c, c], 0)
    Sg = np.concatenate([-s, s, -s, s], 0)
    return np.ascontiguousarray(C), np.ascontiguousarray(Sg)


_NC_CACHE = {}


def kernel(x, c, ctx, c_ctx, w_ada, b_ada, w_in, lam_q1, lam_k1, lam_q2, lam_k2, subln_g,
           w_attn_o, conv_w, conv_b, conv_ln_g, conv_ln_b, w_conv_o, w_out, ln1_g, ln1_b,
           w_router, b_router, w_e_gate, w_e_up, w_e_down, ln2_g, ln2_b, _dbg=False, _stop=None):
    f = lambda a: np.ascontiguousarray(np.asarray(a, dtype=np.float32))
    x, c, ctx, c_ctx = f(x), f(c), f(ctx), f(c_ctx)
    key = (bool(_dbg), _stop)
    if key not in _NC_CACHE:
        _NC_CACHE[key] = build_program(dbg=_dbg, stop=_stop)
    nc = _NC_CACHE[key]
    cols = _perm_cols()
    w_in_r = np.ascontiguousarray(f(w_in)[:, :, cols])
    b_adaT = np.ascontiguousarray(f(b_ada).reshape(2, 48, 128).transpose(2, 0, 1).reshape(128, 96))
    lamv = np.ascontiguousarray(np.stack([f(lam_q1), f(lam_k1), f(lam_q2), f(lam_k2)], 1).reshape(2, 256))
    convw = np.ascontiguousarray(f(conv_w).reshape(2, 31, 4, 128).transpose(3, 0, 2, 1).reshape(128, 248))
    convv = np.ascontiguousarray(np.stack([f(conv_b), f(conv_ln_g), f(conv_ln_b)], 1).reshape(2, 3, 4, 128)
                                 .transpose(3, 0, 1, 2).reshape(128, 24))
    lnv = np.ascontiguousarray(np.stack([f(ln1_g), f(ln1_b), f(ln2_g), f(ln2_b)], 1).reshape(2, 4 * D))
    w_router_r = np.ascontiguousarray(f(w_router).reshape(8, 128, 16).transpose(1, 0, 2).reshape(128, 128))
    shared = dict(identd=np.eye(128, dtype=np.float32), w_ada=f(w_ada), b_adaT=b_adaT, w_in_r=w_in_r, lamv=lamv,
                  sublng=f(subln_g), w_attn_o=f(w_attn_o), w_conv_o=f(w_conv_o), w_out=f(w_out), convw=convw,
                  convv=convv, lnv=lnv, w_router_r=w_router_r, b_router=f(b_router).reshape(1, 16),
                  w_e_gate=f(w_e_gate), w_e_up=f(w_e_up), w_e_down=f(w_e_down))
    if _stop in ('p0', 'A', 'X', 'B', 'C'):
        for kk in ('w_e_gate', 'w_e_up', 'w_e_down'):
            shared[kk] = np.ascontiguousarray(shared[kk][:, 0:1])
    ropes = [_rope_tables(h) for h in range(2)]
    in_maps = []
    for core in range(8):
        b, half = core // 2, core % 2
        cc = np.stack([c[b], c_ctx], 1)
        ccT = np.ascontiguousarray(cc.reshape(8, 128, 2).transpose(1, 0, 2).reshape(128, 16))
        hs = np.zeros((128, 2), np.float32)
        hs[:, 0] = 1.0 if half == 1 else 0.0
        hs[:, 1] = 1.0 if half == 0 else 0.0
        m = dict(shared)
        m.update(x_own=np.ascontiguousarray(x[b, half * NLAT:(half + 1) * NLAT]), ctxb=np.ascontiguousarray(ctx[b]),
                 ccT=ccT, ropeC=ropes[half][0], ropeS=ropes[half][1], halo_sel=hs)
        in_maps.append(m)
    res = run_bass_kernel_spmd(nc, in_maps, core_ids=list(range(8)))
    outp = np.empty((4, 8192, D), np.float32)
    for core in range(8):
        b, half = core // 2, core % 2
        outp[b, half * NLAT:(half + 1) * NLAT] = res.results[core]['out']
    if _dbg:
        return outp, res
    return outp
```

```python
import bisect
import contextlib
import math
import numpy as np
import concourse.bass as bass
import concourse.mybir as mybir
from concourse.bass_utils import run_bass_kernel_spmd

F32 = mybir.dt.float32
BF16 = mybir.dt.bfloat16
U8 = mybir.dt.uint8
AF = mybir.ActivationFunctionType
ALU = mybir.AluOpType
AX = mybir.AxisListType

ENGS = ('pe', 'act', 'dve', 'pool', 'sp')

D = 1024
NLAT = 4096
NCTX = 256
TOK = NLAT + NCTX
NE = 16
DE = 512
INW = 5632
ALPHA = 4.0 ** 0.25
EPS = 1e-5
BROWS = 8256
KROWS = 4096
VROWS = 4128


class Res:
    __slots__ = ('w', 'r', 'name')

    def __init__(self, name=''):
        self.w = {}
        self.r = {}
        self.name = name


class Sched:
    def __init__(self, nc):
        self.nc = nc
        self.progs = {e: [] for e in ENGS}
        self.cnt = {e: 0 for e in ENGS}
        self.known = {e: {} for e in ENGS}
        self.snaps = {e: ([0], [{}]) for e in ENGS}
        self.dma_keys = []
        self.cc_keys = set()
        self.targets = {e: set() for e in ENGS}

    def dma_key(self, name, cc=False):
        k = 'd_' + name
        assert k not in self.cnt
        self.cnt[k] = 0
        self.dma_keys.append(k)
        if cc:
            self.cc_keys.add(k)
        return k

    def _merge_known(self, eng, key, val):
        kn = self.known[eng]
        if kn.get(key, 0) < val:
            kn[key] = val
        if key in self.snaps:
            counts, dicts = self.snaps[key]
            i = bisect.bisect_right(counts, val) - 1
            for k2, v2 in dicts[i].items():
                if kn.get(k2, 0) < v2:
                    kn[k2] = v2

    def _add_waits(self, eng, need):
        kn = self.known[eng]
        waits = [(k, v) for k, v in need.items() if kn.get(k, 0) < v]
        for k, v in waits:
            self._merge_known(eng, k, v)
            if k in self.targets:
                self.targets[k].add(v)
        if waits:
            counts, dicts = self.snaps[eng]
            counts.append(self.cnt[eng] + 1)
            dicts.append(dict(kn))
        return waits

    def op(self, eng, fn, reads=(), writes=(), dma=None):
        need = {}

        def add(clk, same_ok):
            if clk is None:
                return
            k, v = clk
            if k == eng and (eng == 'pe' or not same_ok):
                return
            if need.get(k, 0) < v:
                need[k] = v
        for r in reads:
            for k, v in r.w.items():
                add((k, v), True)
        for w in writes:
            for k, v in w.w.items():
                add((k, v), True)
            for k, v in w.r.items():
                add((k, v), False)
        if dma is not None and self.cnt[dma] > 0:
            if need.get(dma, 0) < self.cnt[dma]:
                need[dma] = self.cnt[dma]
        waits = self._add_waits(eng, need)
        if dma is None:
            self.cnt[eng] += 1
            clk = (eng, self.cnt[eng])
        else:
            self.cnt[dma] += 1
            clk = (dma, self.cnt[dma])
        self.progs[eng].append((fn, waits, clk))
        for r in reads:
            if r.r.get(clk[0], 0) < clk[1]:
                r.r[clk[0]] = clk[1]
        for w in writes:
            if w.w.get(clk[0], 0) < clk[1]:
                w.w[clk[0]] = clk[1]
            w.r = {}
        return clk

    def barrier(self):
        tot = dict(self.cnt)
        for e in ENGS:
            need = {k: v for k, v in tot.items() if k != e and v > 0}
            waits = self._add_waits(e, need)
            if waits:
                self.progs[e].append((None, waits, None))

    def emit(self, final_engine='sp'):
        nc = self.nc
        tot = dict(self.cnt)
        need = {k: v for k, v in tot.items() if k != final_engine and v > 0}
        fw = self._add_waits(final_engine, need)
        if fw:
            self.progs[final_engine].append((None, fw, None))
        tl = {e: sorted(self.targets[e]) for e in ENGS}

        def semval(k, v):
            if k in tl:
                i = bisect.bisect_left(tl[k], v)
                assert i < len(tl[k]) and tl[k][i] == v, (k, v)
                return i + 1
            if k in self.cc_keys:
                return v
            return 16 * v
        keys = list(ENGS) + self.dma_keys
        with contextlib.ExitStack() as st:
            sems = {k: st.enter_context(nc.semaphore('s_' + k)) for k in keys}
            block = st.enter_context(nc.Block())
            handles = {'pe': block.tensor, 'act': block.scalar, 'dve': block.vector,
                       'pool': block.gpsimd, 'sp': block.sync}

            def mk(ename):
                prog = self.progs[ename]
                tset = self.targets[ename]

                def body(e):
                    for fn, waits, clk in prog:
                        for k, v in waits:
                            e.wait_ge(sems[k], semval(k, v))
                        if fn is None:
                            continue
                        ins = fn(e)
                        if clk[0] == ename:
                            if clk[1] in tset:
                                ins.then_inc(sems[ename], 1)
                        elif clk[0] in self.cc_keys:
                            ins.then_inc(sems[clk[0]])
                        else:
                            ins.then_inc(sems[clk[0]], 16)
                return body
            for ename in ENGS:
                handles[ename](mk(ename))
        self.stats = {e: (len(self.progs[e]), len(tl[e])) for e in ENGS}


class Arena:
    def __init__(self, ap_u8, size):
        self.t = ap_u8
        self.size = size
        self.off = 0
        self.marks = []
        self.peak = 0

    def alloc(self, shape_free, dtype, parts=128):
        esz = {F32: 4, BF16: 2}[dtype]
        n = int(np.prod(shape_free))
        nbytes = n * esz
        off = (self.off + 63) // 64 * 64
        assert off + nbytes <= self.size, f'SBUF arena overflow: {off}+{nbytes}>{self.size}'
        self.off = off + nbytes
        self.peak = max(self.peak, self.off)
        ap = self.t[0:parts, off:off + nbytes].bitcast(dtype)
        if len(shape_free) > 1:
            names = ' '.join(f'a{i}' for i in range(len(shape_free)))
            kw = {f'a{i}': int(s) for i, s in enumerate(shape_free)}
            ap = ap.rearrange(f'p ({names}) -> p {names}', **kw)
        return ap

    def mark(self):
        self.marks.append(self.off)

    def release(self):
        self.off = self.marks.pop()


ARENA_BYTES = 200 * 1024


def build_program(dbg=False, n_layers=2, stop=None):
    nc = bass.Bass("TRN2", target_bir_lowering=False)

    def din(name, shape, dt=F32):
        return nc.dram_tensor(name, list(shape), dt, kind="ExternalInput").ap()

    x_own = din("x_own", [NLAT, D])
    ctxb = din("ctxb", [NCTX, D])
    ccT = din("ccT", [128, 16])
    identd = din("identd", [128, 128])
    w_ada = din("w_ada", [2, D, 6 * D])
    b_adaT = din("b_adaT", [128, 96])
    w_in_r = din("w_in_r", [2, D, INW])
    ropeC = din("ropeC", [128, NLAT])
    ropeS = din("ropeS", [128, NLAT])
    lamv = din("lamv", [2, 256])
    sublng = din("sublng", [2, 128])
    w_attn_o = din("w_attn_o", [2, 512, D])
    w_conv_o = din("w_conv_o", [2, 512, D])
    w_out = din("w_out", [2, D, D])
    convw = din("convw", [128, 2 * 4 * 31])
    convv = din("convv", [128, 2 * 3 * 4])
    lnv = din("lnv", [2, 4 * D])
    w_router_r = din("w_router_r", [128, 8 * 16])
    b_router = din("b_router", [1, 16])
    halo_sel = din("halo_sel", [128, 2])
    nexp = 1 if stop in ('p0', 'A', 'X', 'B', 'C') else NE
    w_e_gate = din("w_e_gate", [2, nexp, D, DE])
    w_e_up = din("w_e_up", [2, nexp, D, DE])
    w_e_down = din("w_e_down", [2, nexp, DE, D])
    out = nc.dram_tensor("out", [NLAT, D], F32, kind="ExternalOutput").ap()
    kind_dbg = "ExternalOutput" if dbg else "Internal"
    xs1 = nc.dram_tensor("xs1", [TOK, D], F32, kind=kind_dbg).ap()
    xs2 = nc.dram_tensor("xs2", [TOK, D], F32, kind=kind_dbg).ap()
    gsc = nc.dram_tensor("gsc", [128, 16, TOK], BF16).ap()
    qsc = nc.dram_tensor("qsc", [4, 128, NLAT], BF16).ap()
    ysc = nc.dram_tensor("ysc", [128, 4, NLAT + 32], BF16).ap()
    ycsc = nc.dram_tensor("ycsc", [128, 4, NCTX + 32], BF16).ap()
    ansc = nc.dram_tensor("ansc", [128, 4, TOK], BF16).ap()
    wc_g = nc.dram_tensor("wc_g", [NE, 128, 8 * 512], BF16).ap()
    wc_u = nc.dram_tensor("wc_u", [NE, 128, 8 * 512], BF16).ap()
    wc_d = nc.dram_tensor("wc_d", [NE, 128, 4 * D], BF16).ap()
    wc_3 = nc.dram_tensor("wc_3", [128, 16 * D], BF16).ap()
    CH_ROWS = [1024] * 8 + [32]
    bounce_t = [[nc.dram_tensor(f"bounce{l}_{c}", [CH_ROWS[c], 512], BF16) for c in range(9)] for l in range(2)]
    gath_t = [[nc.dram_tensor(f"gath{l}_{c}", [2 * CH_ROWS[c], 512], BF16) for c in range(9)] for l in range(2)]

    S = Sched(nc)
    st = contextlib.ExitStack()
    with st:
        arena_t = st.enter_context(nc.sbuf_tensor("arena", [128, ARENA_BYTES], U8))
        A = Arena(arena_t.ap() if hasattr(arena_t, 'ap') else arena_t[:, :], ARENA_BYTES)
        PSh = [st.enter_context(nc.psum_tensor(f"ps{i}", [128, 512], F32)) for i in range(4)]
        SCh = [st.enter_context(nc.psum_tensor(f"sc{i}", [128, 1024], F32)) for i in range(2)]
        PS = [p[:, :] for p in PSh]
        SC = [p[:, :] for p in SCh]
        for sc_ in SC:
            PS.append(sc_[:, 0:512])
            PS.append(sc_[:, 512:1024])
        RPS = [Res(f'ps{i}') for i in range(8)]

        def mm(o, lhsT, rhs, start, stop, reads, writes, tp=None, skip=False):
            if tp is None:
                S.op('pe', lambda e: e.matmul(o, lhsT=lhsT, rhs=rhs, start=start, stop=stop,
                                              skip_group_check=skip), reads, writes)
            else:
                S.op('pe', lambda e: e.matmul(o, lhsT=lhsT, rhs=rhs, start=start, stop=stop,
                                              skip_group_check=skip, tile_position=tp), reads, writes)

        def tr(o, in_, reads, writes):
            S.op('pe', lambda e: e.transpose(out=o, in_=in_, identity=identf), reads + [R_const], writes)

        def act(o, in_, func, reads, writes, bias=None, scale=None, accum=None):
            kw = {}
            if bias is not None:
                kw['bias'] = bias
            if scale is not None:
                kw['scale'] = scale
            if accum is not None:
                kw['accum_out'] = accum
            S.op('act', lambda e: e.activation(out=o, in_=in_, func=func, **kw), reads, writes)

        def tt(eng, o, a, b, op, reads, writes):
            S.op(eng, lambda e: e.tensor_tensor(out=o, in0=a, in1=b, op=op), reads, writes)

        def ts(eng, o, a, s1, op0, reads, writes, s2=None, op1=None):
            if op1 is None:
                S.op(eng, lambda e: e.tensor_scalar(out=o, in0=a, scalar1=s1, scalar2=None, op0=op0), reads, writes)
            else:
                S.op(eng, lambda e: e.tensor_scalar(out=o, in0=a, scalar1=s1, scalar2=s2, op0=op0, op1=op1),
                     reads, writes)

        def stt(eng, o, a, s, b, op0, op1, reads, writes):
            S.op(eng, lambda e: e.scalar_tensor_tensor(out=o, in0=a, scalar=s, in1=b, op0=op0, op1=op1),
                 reads, writes)

        def cp(eng, o, a, reads, writes):
            if eng == 'act':
                S.op(eng, lambda e: e.activation(out=o, in_=a, func=AF.Copy), reads, writes)
            else:
                S.op(eng, lambda e: e.tensor_copy(out=o, in_=a), reads, writes)

        def memset(eng, o, v, writes):
            S.op(eng, lambda e: e.memset(o, v), [], writes)

        def recip(o, a, reads, writes):
            S.op('dve', lambda e: e.reciprocal(out=o, in_=a), reads, writes)

        dma_ctr = [0]

        def dma(eng, o, i, reads, writes, key=None):
            if key is None:
                key = KP_misc.get(eng)
            elif isinstance(key, KeyPool):
                key = key.get(eng)
            S.op(eng, lambda e: e.dma_start(out=o, in_=i), reads, writes, dma=key)

        class KeyPool:
            def __init__(self, name, n):
                self.name = name
                self.n = n
                self.keys = {}
                self.i = {}

            def next(self):
                return self

            def get(self, eng):
                if eng not in self.keys:
                    self.keys[eng] = [S.dma_key(f'{self.name}_{eng}{i}') for i in range(self.n)]
                    self.i[eng] = 0
                k = self.keys[eng][self.i[eng] % self.n]
                self.i[eng] += 1
                return k
        KP_ld = KeyPool('ld', 16)
        KP_st = KeyPool('st', 10)
        KP_misc = KeyPool('misc', 4)

        identf = A.alloc([128], F32)
        onesf = A.alloc([128], F32)
        onesm = A.alloc([128], F32)
        epsc = A.alloc([1], F32)
        silucc = A.alloc([16], F32)
        wr_sb = A.alloc([8, 16], F32)
        brt_bc = A.alloc([16], F32)
        hsel = A.alloc([2], F32)
        R_const = Res('const')
        dma('sp', identf, identd, [], [R_const])
        memset('pool', onesf, 1.0, [R_const])
        memset('pool', onesm, 1.0 / 512.0, [R_const])
        memset('pool', epsc, EPS, [R_const])
        dma('sp', silucc, ccT, [], [R_const])
        dma('sp', wr_sb.rearrange('p k e -> p (k e)'), w_router_r, [], [R_const])
        dma('sp', brt_bc, b_router.partition_broadcast(128), [], [R_const])
        dma('sp', hsel, halo_sel, [], [R_const])
        act(silucc, silucc, AF.Silu, [R_const], [R_const])

        modT = A.alloc([48, 2], F32); R_mod = Res('mod')
        modbc = A.alloc([4, D], F32); R_modbc = Res('modbc')
        lnbc = A.alloc([4, D], F32); R_lnbc = Res('lnbc')
        neglam = A.alloc([1], F32); R_lam = Res('lam')
        gsub = A.alloc([128], F32); R_gsub = Res('gsub')
        cw = A.alloc([4, 31], F32); cvv = A.alloc([3, 4], F32); R_cw = Res('cw')
        kcT = A.alloc([4, NCTX], BF16); R_kcT = Res('kcT')
        vcs = A.alloc([2, 4, 129], BF16); R_vcs = Res('vcs')
        qcT = A.alloc([4, NCTX], BF16); R_qcT = Res('qcT')

        blocks_lat = [(512 * b, 512) for b in range(8)]
        blk_ctx = (NLAT, NCTX)

        def x_in_ap(l, t0, n):
            if l == 0:
                if t0 < NLAT:
                    return x_own[t0:t0 + n, :]
                return ctxb[t0 - NLAT:t0 - NLAT + n, :]
            return xs2[t0:t0 + n, :]

        for l in range(n_layers):
            last = (l == 1)
            lam_init = 0.8 - 0.6 * math.exp(-0.3 * l)
            bounce = bounce_t[l]
            gath = gath_t[l]
            S.barrier()
            A.mark()
            A.mark()
            stage = [A.alloc([6 * D], F32) for _ in range(2)]
            R_stage = [Res() for _ in range(2)]
            modacc = A.alloc([96], F32); R_modacc = Res()
            badat = A.alloc([96], F32); R_badat = Res()
            lamt = A.alloc([256], F32); R_lamt = Res()
            lamp = A.alloc([128], F32)
            lams = A.alloc([4], F32)
            tmpb = A.alloc([128], F32); R_tmpb = Res()
            dma('sp', badat, b_adaT, [], [R_badat])
            dma('sp', lamt, lamv[l:l + 1, :].partition_broadcast(128), [], [R_lamt])
            dma('sp', gsub, sublng[l:l + 1, :].partition_broadcast(128), [], [R_gsub])
            dma('sp', lnbc.rearrange('p a d -> p (a d)'), lnv[l:l + 1, :].partition_broadcast(128), [], [R_lnbc])
            dma('sp', cw.rearrange('p c k -> p (c k)'), convw[:, l * 124:(l + 1) * 124], [], [R_cw])
            dma('sp', cvv.rearrange('p a c -> p (a c)'), convv[:, l * 12:(l + 1) * 12], [], [R_cw])
            for k in range(8):
                sb = k % 2
                dma('sp' if k % 2 == 0 else 'pool', stage[sb], w_ada[l, k * 128:(k + 1) * 128, :], [], [R_stage[sb]],
                    key=KP_ld.next())
                for ch in range(48):
                    mm(PS[0][:, 2 * ch:2 * ch + 2], stage[sb][:, ch * 128:(ch + 1) * 128], silucc[:, 2 * k:2 * k + 2],
                       True, True, [R_stage[sb], R_const], [RPS[0]])
                if k == 0:
                    cp('dve', modacc, PS[0][:, 0:96], [RPS[0]], [R_modacc])
                else:
                    tt('dve', modacc, modacc, PS[0][:, 0:96], ALU.add, [RPS[0], R_modacc], [R_modacc])
            macc3 = modacc.rearrange('p (c j) -> p c j', j=2)
            for j in range(2):
                tt('dve', modT[:, :, j], macc3[:, :, j], badat[:, l * 48:(l + 1) * 48], ALU.add,
                   [R_modacc, R_badat], [R_mod])
            for mi in (1, 4):
                ts('dve', modT[:, mi * 8:(mi + 1) * 8, :], modT[:, mi * 8:(mi + 1) * 8, :], 1.0, ALU.add, [R_mod], [R_mod])
            for bi, (mi, j) in enumerate([(2, 0), (2, 1), (5, 0), (5, 1)]):
                for kh in range(2):
                    for k4 in range(4):
                        k = kh * 4 + k4
                        ts('dve', tmpb, onesf, modT[:, mi * 8 + k, j:j + 1], ALU.mult, [R_mod, R_const, R_tmpb], [R_tmpb])
                        mm(PS[1][:, k4 * 128:(k4 + 1) * 128], tmpb, identf, True, True, [R_tmpb, R_const], [RPS[1]])
                    cp('dve', modbc[:, bi, kh * 512:(kh + 1) * 512], PS[1], [RPS[1]], [R_modbc])
            for i in range(2):
                tt('dve', lamp[:, 0:64], lamt[:, 128 * i:128 * i + 64], lamt[:, 128 * i + 64:128 * i + 128], ALU.mult,
                   [R_lamt, R_lam], [R_lam])
                S.op('dve', lambda e, i=i: e.reduce_sum(out=lams[:, i:i + 1], in_=lamp[:, 0:64], axis=AX.X),
                     [R_lam], [R_lam])
            act(lams[:, 0:2], lams[:, 0:2], AF.Exp, [R_lam], [R_lam])
            tt('dve', lams[:, 2:3], lams[:, 1:2], lams[:, 0:1], ALU.subtract, [R_lam], [R_lam])
            ts('dve', neglam, lams[:, 2:3], -lam_init, ALU.add, [R_lam], [R_lam])
            ts('dve', gsub, gsub, 1.0 - lam_init, ALU.mult, [R_gsub], [R_gsub])
            S.barrier()
            A.release()
            if stop == 'p0':
                S.emit(); print('sched stats', S.stats); return nc

            A.mark()
            R_qT = Res('qsc'); R_yT = Res('ysc'); R_ycT = Res('ycsc')
            A.mark()
            zt = A.alloc([4, 16], BF16); R_zt = Res()
            hxT = A.alloc([8, TOK], BF16)
            R_hx = [Res(f'hx{t}') for t in range(34)]
            xt_b = [A.alloc([D], F32) for _ in range(2)]; R_xt = [Res() for _ in range(2)]
            wst = [A.alloc([8, 256], F32) for _ in range(2)]; R_wst = [Res() for _ in range(2)]
            wbf = [A.alloc([8, 256], BF16) for _ in range(2)]; R_wbf = [Res() for _ in range(2)]
            rC = [A.alloc([512], F32) for _ in range(2)]; rS = [A.alloc([512], F32) for _ in range(2)]
            R_rope = [Res() for _ in range(2)]
            t1 = [A.alloc([512], F32) for _ in range(2)]; R_t1 = [Res() for _ in range(2)]
            t2 = [A.alloc([512], F32) for _ in range(2)]; R_t2 = [Res() for _ in range(2)]
            ot = [A.alloc([2, 512], BF16) for _ in range(3)]; R_ot = [Res() for _ in range(3)]
            vt = [A.alloc([2, 128], BF16) for _ in range(3)]; R_vt = [Res() for _ in range(3)]
            memset('pool', zt, 0.0, [R_zt])
            dma('pool', ycsc[:, :, 0:16], zt, [R_zt], [R_ycT])
            dma('pool', ycsc[:, :, NCTX + 16:NCTX + 32], zt, [R_zt], [R_ycT])
            for t in range(34):
                j = 0 if t < 32 else 1
                xb = t % 2
                dma('sp', xt_b[xb], x_in_ap(l, t * 128, 128), [], [R_xt[xb]], key=KP_ld.next())
                for kh in range(2):
                    pb = 2 + (2 * t + kh) % 2
                    for k4 in range(4):
                        k = kh * 4 + k4
                        tr(PS[pb][:, k4 * 128:(k4 + 1) * 128], xt_b[xb][:, k * 128:(k + 1) * 128], [R_xt[xb]], [RPS[pb]])
                    for k4 in range(4):
                        k = kh * 4 + k4
                        act(hxT[:, k, t * 128:(t + 1) * 128], PS[pb][:, k4 * 128:(k4 + 1) * 128], AF.Identity,
                            [RPS[pb], R_mod], [R_hx[t]], bias=modT[:, 0 * 8 + k, j:j + 1], scale=modT[:, 1 * 8 + k, j:j + 1])
            kreg = [bounce[h][:, :].rearrange('(p a) b -> p (a b)', p=128, a=8) for h in range(4)]
            ereg = bounce[8][:, :].rearrange('a b -> (a b)').rearrange(
                '(p c s t) -> p c s t', p=128, c=4, s=2, t=16)
            R_bounce = Res('bounce')
            R_gsc = Res('gsc')
            groups = ([('Q', h) for h in range(4)] + [('K', h) for h in range(4)] + [('V', g) for g in range(2)]
                      + [('U', c) for c in range(4)] + [('G', g) for g in range(8)])
            tok_blocks = blocks_lat + [blk_ctx]
            oti = 0
            vti = 0
            pbi = 0
            for gi, (kind, idx) in enumerate(groups):
                wb = gi % 2
                c0 = gi * 256
                dma('sp' if gi % 2 == 0 else 'pool', wst[wb],
                    w_in_r[l, :, c0:c0 + 256].rearrange('(k p) c -> p k c', p=128), [], [R_wst[wb]], key=KP_ld.next())
                cp('pool', wbf[wb], wst[wb], [R_wst[wb]], [R_wbf[wb]])
                for bi_, (t0, n) in enumerate(tok_blocks):
                    isctx = t0 >= NLAT
                    if isctx and last and kind in ('Q', 'U', 'G'):
                        continue
                    hx_res = [R_hx[t0 // 128 + i] for i in range(n // 128)]
                    if kind in ('Q', 'K', 'U', 'G'):
                        pa, pbk = 4 + (pbi % 2) * 2, 5 + (pbi % 2) * 2
                        pbi += 1
                        need_b = not (isctx and kind in ('Q', 'K'))
                        for half, pidx in ((0, pa), (1, pbk)):
                            if half == 1 and not need_b:
                                continue
                            for k in range(8):
                                mm(PS[pidx][:, 0:n], wbf[wb][:, k, half * 128:(half + 1) * 128], hxT[:, k, t0:t0 + n],
                                   k == 0, k == 7, [R_wbf[wb]] + hx_res, [RPS[pidx]])
                        if kind in ('Q', 'K'):
                            if isctx:
                                dst, rdst = (qcT, R_qcT) if kind == 'Q' else (kcT, R_kcT)
                                cp('dve', dst[:, idx, :], PS[pa][:, 0:n], [RPS[pa]], [rdst])
                            else:
                                rb = bi_ % 2
                                if gi == 0 or True:
                                    dma('sp', rC[rb], ropeC[:, t0:t0 + 512], [], [R_rope[rb]], key=KP_ld.next())
                                    dma('sp', rS[rb], ropeS[:, t0:t0 + 512], [], [R_rope[rb]], key=KP_ld.next())
                                tb = bi_ % 2
                                tt('dve', t1[tb], PS[pa], rC[rb], ALU.mult, [RPS[pa], R_rope[rb]], [R_t1[tb]])
                                tt('dve', t2[tb], PS[pbk], rS[rb], ALU.mult, [RPS[pbk], R_rope[rb]], [R_t2[tb]])
                                o = oti % 3
                                oti += 1
                                tt('pool', ot[o][:, 0, :], t1[tb], t2[tb], ALU.add, [R_t1[tb], R_t2[tb]], [R_ot[o]])
                                if kind == 'Q':
                                    dma('pool', qsc[idx, :, t0:t0 + 512], ot[o][:, 0, :], [R_ot[o]], [R_qT],
                                        key=KP_st.next())
                                else:
                                    dma('pool', kreg[idx][:, t0:t0 + 512], ot[o][:, 0, :], [R_ot[o]], [R_bounce],
                                        key=KP_st.next())
                        elif kind == 'U':
                            tb = bi_ % 2
                            act(t1[tb][:, 0:n], PS[pbk][:, 0:n], AF.Sigmoid, [RPS[pbk]], [R_t1[tb]])
                            o = oti % 3
                            oti += 1
                            tt('dve', ot[o][:, 0, 0:n], PS[pa][:, 0:n], t1[tb][:, 0:n], ALU.mult,
                               [RPS[pa], R_t1[tb]], [R_ot[o]])
                            if isctx:
                                dma('pool', ycsc[:, idx, 16:16 + n], ot[o][:, 0, 0:n], [R_ot[o]], [R_ycT], key=KP_st.next())
                            else:
                                dma('pool', ysc[:, idx, 16 + t0:16 + t0 + n], ot[o][:, 0, 0:n], [R_ot[o]], [R_yT],
                                    key=KP_st.next())
                        else:
                            o = oti % 3
                            oti += 1
                            act(ot[o][:, 0, 0:n], PS[pa][:, 0:n], AF.Sigmoid, [RPS[pa]], [R_ot[o]])
                            act(ot[o][:, 1, 0:n], PS[pbk][:, 0:n], AF.Sigmoid, [RPS[pbk]], [R_ot[o]])
                            dma('pool', gsc[:, 2 * idx:2 * idx + 2, t0:t0 + n], ot[o][:, :, 0:n], [R_ot[o]], [R_gsc],
                                key=KP_st.next())
                    else:
                        for tl_ in range(n // 128):
                            tk0 = t0 + tl_ * 128
                            pv = 4 + (pbi % 4)
                            pbi += 1
                            for k in range(8):
                                mm(PS[pv][:, 0:256], hxT[:, k, tk0:tk0 + 128], wbf[wb][:, k, :], k == 0, k == 7,
                                   [R_wbf[wb], R_hx[tk0 // 128]], [RPS[pv]])
                            if isctx:
                                cp('dve', vcs[:, tl_, 2 * idx:2 * idx + 2, 0:128],
                                   PS[pv][:, 0:256].rearrange('p (h e) -> p h e', h=2), [RPS[pv]], [R_vcs])
                            else:
                                v = vti % 3
                                vti += 1
                                cp('dve', vt[v][:, :, 0:128], PS[pv][:, 0:256].rearrange('p (h e) -> p h e', h=2),
                                   [RPS[pv]], [R_vt[v]])
                                dma('pool', bounce[4 + tk0 // 1024][tk0 % 1024:tk0 % 1024 + 128, idx * 256:(idx + 1) * 256],
                                    vt[v].rearrange('p h e -> p (h e)'), [R_vt[v]], [R_bounce], key=KP_st.next())
            memset('pool', vcs[:, :, :, 128:129], 1.0, [R_vcs])
            dma('pool', ereg[:, :, 0, :], ysc[:, :, 16:32], [R_yT], [R_bounce], key=KP_st.next())
            dma('pool', ereg[:, :, 1, :], ysc[:, :, NLAT:NLAT + 16], [R_yT], [R_bounce], key=KP_st.next())
            if stop == 'A':
                S.emit(); print('sched stats', S.stats); return nc
            R_gc = [Res(f'gath{c}') for c in range(9)]
            for c in range(9):
                cckey = S.dma_key(f'cc{l}_{c}', cc=True)
                S.op('pool', lambda e, b_=bounce[c], g_=gath[c]: e.collective_compute(
                    "AllGather", ALU.bypass, replica_groups=[[0, 1], [2, 3], [4, 5], [6, 7]],
                    ins=[b_.ap().opt()], outs=[g_.ap().opt()]), [R_bounce], [R_gc[c]], dma=cckey)
            S.barrier()
            A.release()
            A.mark()
            egs = [gath[8][r * 32:(r + 1) * 32, :].rearrange('a b -> (a b)').rearrange(
                '(p c s t) -> p c s t', p=128, c=4, s=2, t=16) for r in range(2)]
            dma('sp', ysc[:, :, 0:16], egs[0][:, :, 1, :], [R_gc[8]], [R_yT])
            dma('sp', ysc[:, :, NLAT + 16:NLAT + 32], egs[1][:, :, 0, :], [R_gc[8]], [R_yT])

            if stop == 'X':
                S.emit(); print('sched stats', S.stats); return nc
            R_anT = Res('ansc')
            A.mark()
            anst = [A.alloc([512], BF16) for _ in range(2)]; R_anst = [Res() for _ in range(2)]
            qblk = [A.alloc([512], BF16) for _ in range(2)]; R_qblk = [Res() for _ in range(2)]
            qbi = 0
            kT = [A.alloc([2 * NLAT], BF16) for _ in range(2)]; R_kT = [Res() for _ in range(2)]
            vh = [A.alloc([64, 129], BF16) for _ in range(2)]; R_vh = [Res() for _ in range(2)]
            pT = [A.alloc([1024], BF16) for _ in range(3)]; R_pT = [Res() for _ in range(3)]
            for i in range(2):
                memset('pool', vh[i][:, :, 128:129], 1.0, [R_vh[i]])
            nrm = A.alloc([8], F32); R_nrm = Res()
            tO = A.alloc([128], F32); oO = A.alloc([128], F32); onf = A.alloc([128], F32); R_o = Res()
            junk = A.alloc([128], F32)
            pti = 0
            sci = 0
            pst = [A.alloc([4, 512], F32) for _ in range(3)]; R_pst = [Res() for _ in range(3)]
            pbf = [A.alloc([4, 512], BF16) for _ in range(2)]; R_pbf = [Res() for _ in range(2)]
            R_wc3 = Res('wc3')
            R_wc = [Res(f'wc{e_}') for e_ in range(NE)]
            wc_3v = wc_3.rearrange('p (a d) -> p a d', a=16)
            pieces = []
            for hh in range(2):
                pieces.append((w_attn_o[l, :, hh * 512:(hh + 1) * 512].rearrange('(k p) c -> p k c', p=128),
                               wc_3v[:, 0:4, hh * 512:(hh + 1) * 512], R_wc3))
                pieces.append((w_conv_o[l, :, hh * 512:(hh + 1) * 512].rearrange('(k p) c -> p k c', p=128),
                               wc_3v[:, 4:8, hh * 512:(hh + 1) * 512], R_wc3))
                for kh in range(2):
                    pieces.append((w_out[l, kh * 512:(kh + 1) * 512, hh * 512:(hh + 1) * 512].rearrange('(k p) c -> p k c', p=128),
                                   wc_3v[:, 8 + kh * 4:8 + (kh + 1) * 4, hh * 512:(hh + 1) * 512], R_wc3))
            for ex in range(NE):
                wgv = wc_g[ex].rearrange('p (k c) -> p k c', k=8)
                wuv = wc_u[ex].rearrange('p (k c) -> p k c', k=8)
                wdv = wc_d[ex].rearrange('p (k c) -> p k c', k=4)
                for kh in range(2):
                    pieces.append((w_e_gate[l, ex, kh * 512:(kh + 1) * 512, :].rearrange('(k p) c -> p k c', p=128),
                                   wgv[:, kh * 4:(kh + 1) * 4, :], R_wc[ex]))
                    pieces.append((w_e_up[l, ex, kh * 512:(kh + 1) * 512, :].rearrange('(k p) c -> p k c', p=128),
                                   wuv[:, kh * 4:(kh + 1) * 4, :], R_wc[ex]))
                for hh in range(2):
                    pieces.append((w_e_down[l, ex, :, hh * 512:(hh + 1) * 512].rearrange('(k p) c -> p k c', p=128),
                                   wdv[:, :, hh * 512:(hh + 1) * 512], R_wc[ex]))
            pci = [0]

            def emit_pieces(cnt):
                for _ in range(cnt):
                    if pci[0] >= len(pieces):
                        return
                    src, dst, rdst = pieces[pci[0]]
                    i3 = pci[0] % 3
                    i2 = pci[0] % 2
                    pci[0] += 1
                    dma('sp', pst[i3], src, [], [R_pst[i3]], key=KP_ld.next())
                    cp('pool' if pci[0] % 3 else 'dve', pbf[i2], pst[i3], [R_pst[i3]], [R_pbf[i2]])
                    dma('pool', dst, pbf[i2], [R_pbf[i2]], [rdst], key=KP_st.next())
            for h in range(4):
                hb = h % 2
                for r in range(2):
                    kg = gath[h][r * 1024:(r + 1) * 1024, :].rearrange('(p a) b -> p (a b)', p=128, a=8)
                    dma('sp', kT[hb][:, r * NLAT:(r + 1) * NLAT], kg, [R_gc[h]], [R_kT[hb]], key=KP_ld.next())
                    for q4 in range(4):
                        vg = gath[4 + q4][r * 1024:(r + 1) * 1024, h * 128:(h + 1) * 128].rearrange('(t p) e -> p t e', p=128)
                        dma('pool', vh[hb][:, r * 32 + q4 * 8:r * 32 + q4 * 8 + 8, 0:128],
                            vg, [R_gc[4 + q4]], [R_vh[hb]], key=KP_ld.next())
                qblocks = [(t0, n, False) for (t0, n) in blocks_lat]
                if not last:
                    qblocks.append((NLAT, NCTX, True))
                for (t0, n, isctx) in qblocks:
                    nqt = n // 128
                    qb_ = qbi % 2
                    qbi += 1
                    if not isctx:
                        dma('sp', qblk[qb_], qsc[h, :, t0:t0 + n], [R_qT], [R_qblk[qb_]], key=KP_ld.next())
                    ktiles = [('c', i) for i in range(2)]
                    if not isctx:
                        ktiles += [('l', i) for i in range(64)]
                    def emit_qk(kti):
                        kk, ki = ktiles[kti]
                        sb0 = 4 + (kti % 2) * 2
                        for m in range(2):
                            if kk == 'c':
                                lh = kcT[64 * m:64 * m + 64, h, ki * 128:(ki + 1) * 128]
                                rr = [R_kcT]
                            else:
                                lh = kT[hb][64 * m:64 * m + 64, ki * 128:(ki + 1) * 128]
                                rr = [R_kT[hb]]
                            if isctx:
                                rh = qcT[64 * m:64 * m + 64, h, :]
                                rr = rr + [R_qcT]
                            else:
                                rh = qblk[qb_][64 * m:64 * m + 64, 0:n]
                                rr = rr + [R_qblk[qb_]]
                            mm(PS[sb0 + m][:, 0:n], lh, rh, True, True, rr, [RPS[sb0 + m]], tp=(64 * m, 0))
                    emit_qk(0)
                    for kti, (kk, ki) in enumerate(ktiles):
                        sb0 = 4 + (kti % 2) * 2
                        if kti + 1 < len(ktiles):
                            emit_qk(kti + 1)
                        p = pti % 3
                        pti += 1
                        act(pT[p].rearrange('p (m q) -> p m q', m=2)[:, :, 0:n],
                            SC[(sb0 - 4) // 2].rearrange('p (m q) -> p m q', m=2)[:, :, 0:n], AF.Exp,
                            [RPS[sb0], RPS[sb0 + 1]], [R_pT[p]], scale=0.125)
                        for qt in range(nqt):
                            for m in range(2):
                                if kk == 'c':
                                    vv = vcs[:, ki, h, :]
                                    rv = [R_vcs]
                                else:
                                    vv = vh[hb][:, ki, :]
                                    rv = [R_vh[hb]]
                                mm(PS[qt][:, m * 129:(m + 1) * 129], pT[p][:, m * 512 + qt * 128:m * 512 + qt * 128 + 128], vv,
                                   (kti == 0 and m == 0), kti == len(ktiles) - 1, [R_pT[p]] + rv, [RPS[qt]], skip=True)
                    for qt in range(nqt):
                        acc = PS[qt]
                        recip(nrm[:, 0:1], acc[:, 128:129], [RPS[qt], R_nrm], [R_nrm])
                        recip(nrm[:, 1:2], acc[:, 257:258], [RPS[qt], R_nrm], [R_nrm])
                        tt('dve', nrm[:, 2:3], nrm[:, 1:2], neglam, ALU.mult, [R_nrm, R_lam], [R_nrm])
                        ts('dve', tO, acc[:, 129:257], nrm[:, 2:3], ALU.mult, [RPS[qt], R_nrm, R_o], [R_o])
                        stt('dve', oO, acc[:, 0:128], nrm[:, 0:1], tO, ALU.mult, ALU.add, [RPS[qt], R_nrm, R_o], [R_o])
                        memset('dve', nrm[:, 3:4], 0.0, [R_nrm])
                        act(junk, oO, AF.Square, [R_o], [R_o, R_nrm], accum=nrm[:, 3:4])
                        act(nrm[:, 4:5], nrm[:, 3:4], AF.Sqrt, [R_nrm, R_const], [R_nrm], bias=epsc, scale=1.0 / 128.0)
                        recip(nrm[:, 5:6], nrm[:, 4:5], [R_nrm], [R_nrm])
                        stt('dve', onf, oO, nrm[:, 5:6], gsub, ALU.mult, ALU.mult, [R_o, R_nrm, R_gsub], [R_o])
                        tr(PS[4][:, qt * 128:(qt + 1) * 128], onf, [R_o], [RPS[4]])
                        cp('dve', anst[qb_][:, qt * 128:(qt + 1) * 128], PS[4][:, qt * 128:(qt + 1) * 128],
                           [RPS[4]], [R_anst[qb_]])
                    dma('sp', ansc[:, h, t0:t0 + n], anst[qb_][:, 0:n], [R_anst[qb_]], [R_anT], key=KP_st.next())
                    emit_pieces(4)
            emit_pieces(len(pieces))
            S.barrier()
            A.release()

            if stop == 'B':
                S.emit(); print('sched stats', S.stats); return nc
            A.mark()
            wst2 = [A.alloc([4, 512], F32) for _ in range(3)]; R_wst2 = [Res() for _ in range(3)]
            wsi = [0]

            def load_cast(dst_ap, src_ap, rdst):
                i = wsi[0] % 3
                wsi[0] += 1
                dma('sp', wst2[i], src_ap, [], [R_wst2[i]], key=KP_ld.next())
                if wsi[0] % 3 == 0:
                    cp('act', dst_ap, wst2[i], [R_wst2[i]], [rdst])
                else:
                    cp('pool', dst_ap, wst2[i], [R_wst2[i]], [rdst])
            SBT = 512
            NST = SBT // 128
            hx2T = A.alloc([8, SBT], BF16); R_hx2 = [Res() for _ in range(NST)]
            macc = A.alloc([NST, D], F32); R_macc = [Res() for _ in range(NST)]
            gts = A.alloc([NST, 16], F32); R_gts = [Res() for _ in range(NST)]
            xin = [A.alloc([D], F32) for _ in range(2)]; R_xin = [Res() for _ in range(2)]
            yb = [A.alloc([D], F32) for _ in range(2)]; R_yb = [Res() for _ in range(2)]
            stat = A.alloc([12], F32); mv = A.alloc([4], F32); R_mv = Res()
            hx2f = A.alloc([8, 128], F32); R_hx2f = Res()
            rt = A.alloc([16 * 8], F32); R_rt = Res()
            xi_i = [0]

            def layer_norm_tile(yt, ry, gi, dst_dram, rdst):
                for c2 in range(2):
                    S.op('dve', lambda e, c2=c2: e.bn_stats(out=stat[:, c2 * 6:(c2 + 1) * 6], in_=yt[:, c2 * 512:(c2 + 1) * 512]),
                         [ry], [R_mv])
                S.op('dve', lambda e: e.bn_aggr(out=mv[:, 0:2], in_=stat), [R_mv], [R_mv])
                act(mv[:, 2:3], mv[:, 1:2], AF.Sqrt, [R_mv, R_const], [R_mv], bias=epsc, scale=1.0)
                recip(mv[:, 3:4], mv[:, 2:3], [R_mv], [R_mv])
                ts('dve', yt, yt, mv[:, 0:1], ALU.subtract, [ry, R_mv], [ry], s2=mv[:, 3:4], op1=ALU.mult)
                tt('pool', yt, yt, lnbc[:, gi, :], ALU.mult, [ry, R_lnbc], [ry])
                tt('pool', yt, yt, lnbc[:, gi + 1, :], ALU.add, [ry, R_lnbc], [ry])
                dma('sp', dst_dram, yt, [ry], [rdst], key=KP_st.next())

            R_xs1 = Res('xs1')
            R_out = Res('out')
            all_blocks = list(blocks_lat) + ([blk_ctx] if not last else [])
            for (t0, n) in all_blocks:
                isctx = t0 >= NLAT
                j = 1 if isctx else 0
                A.mark()
                w3all = A.alloc([16, D], BF16)
                wao = w3all[:, 0:4, :]; wco = w3all[:, 4:8, :]; wo = w3all[:, 8:16, :]
                R_w3 = Res('w3')
                first_blk = False
                if not first_blk:
                    dma('sp', w3all.rearrange('p a d -> p (a d)'), wc_3, [R_wc3], [R_w3], key=KP_ld.next())
                for hh in (range(2) if first_blk else []):
                    load_cast(wao[:, :, hh * 512:(hh + 1) * 512],
                              w_attn_o[l, :, hh * 512:(hh + 1) * 512].rearrange('(k p) c -> p k c', p=128), R_w3)
                    load_cast(wco[:, :, hh * 512:(hh + 1) * 512],
                              w_conv_o[l, :, hh * 512:(hh + 1) * 512].rearrange('(k p) c -> p k c', p=128), R_w3)
                    for kh in range(2):
                        load_cast(wo[:, kh * 4:(kh + 1) * 4, hh * 512:(hh + 1) * 512],
                                  w_out[l, kh * 512:(kh + 1) * 512, hh * 512:(hh + 1) * 512].rearrange('(k p) c -> p k c', p=128),
                                  R_w3)
                if first_blk:
                    dma('pool', wc_3, w3all.rearrange('p a d -> p (a d)'), [R_w3], [R_wc3], key=KP_st.next())
                z = A.alloc([4, 512], F32); R_z = [Res() for _ in range(4)]
                zq = A.alloc([512], F32); R_zq = Res()
                mean = A.alloc([512], F32); rstd = A.alloc([512], F32); R_st = Res()
                snT = A.alloc([4, 512], BF16); R_snT = Res()
                gtl = [A.alloc([2, 512], BF16) for _ in range(2)]; R_gtl = [Res() for _ in range(2)]
                GT = A.alloc([8, 512], BF16); R_GT = Res()
                tg1 = A.alloc([512], F32); tg2 = A.alloc([512], F32); R_tg = Res()
                yblk = A.alloc([4, 512 + 32], BF16); R_yblk = Res()
                anblk = A.alloc([4, 512], BF16); R_anblk = Res()
                if isctx:
                    dma('sp', yblk[:, :, 0:n + 32], ycsc[:, :, 0:n + 32], [R_ycT], [R_yblk], key=KP_ld.next())
                else:
                    dma('sp', yblk[:, :, 0:n + 32], ysc[:, :, t0:t0 + n + 32], [R_yT], [R_yblk], key=KP_ld.next())
                    if t0 == 0:
                        ts('dve', yblk[:, :, 0:16], yblk[:, :, 0:16], hsel[:, 0:1], ALU.mult, [R_yblk, R_const], [R_yblk])
                    if t0 + n == NLAT:
                        ts('dve', yblk[:, :, n + 16:n + 32], yblk[:, :, n + 16:n + 32], hsel[:, 1:2], ALU.mult,
                           [R_yblk, R_const], [R_yblk])
                dma('sp', anblk[:, :, 0:n], ansc[:, :, t0:t0 + n], [R_anT], [R_anblk], key=KP_ld.next())
                for c in range(4):
                    ts('dve', z[:, c, 0:n], yblk[:, c, 1:1 + n], cw[:, c, 0:1], ALU.mult,
                       [R_yblk, R_cw], [R_z[c]], s2=cvv[:, 0, c:c + 1], op1=ALU.add)
                    for k in range(1, 31):
                        stt('dve', z[:, c, 0:n], yblk[:, c, k + 1:k + 1 + n], cw[:, c, k:k + 1], z[:, c, 0:n],
                            ALU.mult, ALU.add, [R_yblk, R_cw], [R_z[c]])
                for c in range(4):
                    mm(PS[0][:, 0:n], onesm, z[:, c, 0:n], c == 0, c == 3, [R_const, R_z[c]], [RPS[0]])
                for c in range(4):
                    act(zq[:, 0:n], z[:, c, 0:n], AF.Square, [R_z[c]], [R_zq])
                    mm(PS[1][:, 0:n], onesm, zq[:, 0:n], c == 0, c == 3, [R_const, R_zq], [RPS[1]])
                cp('dve', mean[:, 0:n], PS[0][:, 0:n], [RPS[0]], [R_st])
                tt('dve', rstd[:, 0:n], mean[:, 0:n], mean[:, 0:n], ALU.mult, [R_st], [R_st])
                tt('dve', rstd[:, 0:n], PS[1][:, 0:n], rstd[:, 0:n], ALU.subtract, [RPS[1], R_st], [R_st])
                act(rstd[:, 0:n], rstd[:, 0:n], AF.Sqrt, [R_st, R_const], [R_st], bias=epsc, scale=1.0)
                recip(rstd[:, 0:n], rstd[:, 0:n], [R_st], [R_st])
                for c in range(4):
                    tt('dve', z[:, c, 0:n], z[:, c, 0:n], mean[:, 0:n], ALU.subtract, [R_st, R_z[c]], [R_z[c]])
                    tt('dve', z[:, c, 0:n], z[:, c, 0:n], rstd[:, 0:n], ALU.mult, [R_st, R_z[c]], [R_z[c]])
                    act(snT[:, c, 0:n], z[:, c, 0:n], AF.Silu, [R_z[c], R_cw], [R_snT],
                        bias=cvv[:, 2, c:c + 1], scale=cvv[:, 1, c:c + 1])
                for oc in range(8):
                    pa2 = 2 + (oc % 2) * 2
                    g_i = oc % 2
                    dma('sp', gtl[g_i][:, 0, 0:n], gsc[:, oc, t0:t0 + n], [R_gsc], [R_gtl[g_i]], key=KP_ld.next())
                    dma('sp', gtl[g_i][:, 1, 0:n], gsc[:, 8 + oc, t0:t0 + n], [R_gsc], [R_gtl[g_i]], key=KP_ld.next())
                    for hh in range(4):
                        mm(PS[pa2][:, 0:n], wao[:, hh, oc * 128:(oc + 1) * 128], anblk[:, hh, 0:n], hh == 0, hh == 3,
                           [R_w3, R_anblk], [RPS[pa2]])
                    for c in range(4):
                        mm(PS[pa2 + 1][:, 0:n], wco[:, c, oc * 128:(oc + 1) * 128], snT[:, c, 0:n], c == 0, c == 3,
                           [R_w3, R_snT], [RPS[pa2 + 1]])
                    tt('dve', tg1[:, 0:n], PS[pa2][:, 0:n], gtl[g_i][:, 0, 0:n], ALU.mult,
                       [RPS[pa2], R_gtl[g_i], R_tg], [R_tg])
                    tt('dve', tg2[:, 0:n], PS[pa2 + 1][:, 0:n], gtl[g_i][:, 1, 0:n], ALU.mult,
                       [RPS[pa2 + 1], R_gtl[g_i], R_tg], [R_tg])
                    tt('pool', GT[:, oc, 0:n], tg1[:, 0:n], tg2[:, 0:n], ALU.add, [R_tg], [R_GT])
                for tl_ in range(n // 128):
                    tk0 = t0 + tl_ * 128
                    sti = tl_
                    xi = xi_i[0] % 2
                    xi_i[0] += 1
                    dma('sp', xin[xi], x_in_ap(l, tk0, 128), [], [R_xin[xi]], key=KP_ld.next())
                    for hf in range(2):
                        pm = 6 + hf
                        for oc in range(8):
                            mm(PS[pm], GT[:, oc, tl_ * 128:(tl_ + 1) * 128], wo[:, oc, hf * 512:(hf + 1) * 512],
                               oc == 0, oc == 7, [R_GT, R_w3], [RPS[pm]])
                        tt('dve', yb[xi][:, hf * 512:(hf + 1) * 512], PS[pm], modbc[:, 0 + j, hf * 512:(hf + 1) * 512],
                           ALU.mult, [RPS[pm], R_modbc], [R_yb[xi]])
                    stt('dve', yb[xi], xin[xi], ALPHA, yb[xi], ALU.mult, ALU.add, [R_xin[xi], R_yb[xi]], [R_yb[xi]])
                    layer_norm_tile(yb[xi], R_yb[xi], 0, xs1[tk0:tk0 + 128, :], R_xs1)
                    for kh in range(2):
                        pb = 0 + kh
                        for k4 in range(4):
                            k = kh * 4 + k4
                            tr(PS[pb][:, k4 * 128:(k4 + 1) * 128], yb[xi][:, k * 128:(k + 1) * 128], [R_yb[xi]], [RPS[pb]])
                        for k4 in range(4):
                            k = kh * 4 + k4
                            act(hx2f[:, k, :], PS[pb][:, k4 * 128:(k4 + 1) * 128], AF.Identity, [RPS[pb], R_mod], [R_hx2f],
                                bias=modT[:, 3 * 8 + k, j:j + 1], scale=modT[:, 4 * 8 + k, j:j + 1])
                    cp('pool', hx2T[:, :, sti * 128:(sti + 1) * 128], hx2f, [R_hx2f], [R_hx2[sti]])
                    for k in range(8):
                        mm(PS[0][:, 0:16], hx2f[:, k, :], wr_sb[:, k, :], k == 0, k == 7, [R_hx2f, R_const], [RPS[0]])
                    lg = rt[:, 0:16]; pr = rt[:, 16:32]; pm6 = rt[:, 32:56]; gsc4 = rt[:, 56:60]
                    sc1 = rt[:, 60:61]; sc2 = rt[:, 61:62]; sc3 = rt[:, 62:63]; sc4 = rt[:, 63:64]
                    gm = rt[:, 64:68]; mk = rt[:, 68:84]; mk2 = rt[:, 84:100]; ok16 = rt[:, 100:116]
                    RR = [R_rt]
                    tt('dve', lg, PS[0][:, 0:16], brt_bc, ALU.add, [RPS[0], R_const, R_rt], RR)
                    S.op('dve', lambda e, lg=lg, sc1=sc1: e.reduce_max(out=sc1, in_=lg, axis=AX.X), RR, RR)
                    ts('dve', sc2, sc1, -1.0, ALU.mult, RR, RR)
                    memset('dve', sc3, 0.0, RR)
                    act(pr, lg, AF.Exp, RR, RR, bias=sc2, scale=1.0, accum=sc3)
                    recip(sc4, sc3, RR, RR)
                    ts('dve', pr, pr, sc4, ALU.mult, RR, RR)
                    pr4 = pr.rearrange('p (g e) -> p g e', e=4)
                    pm64 = pm6.rearrange('p (g s) -> p g s', s=6)
                    for si, (a_, b_) in enumerate([(0, 1), (0, 2), (0, 3), (1, 2), (1, 3), (2, 3)]):
                        tt('dve', pm64[:, :, si], pr4[:, :, a_], pr4[:, :, b_], ALU.add, RR, RR)
                    S.op('dve', lambda e, pm64=pm64, gsc4=gsc4: e.tensor_reduce(out=gsc4, in_=pm64, axis=AX.X, op=ALU.max),
                         RR, RR)
                    S.op('dve', lambda e, gsc4=gsc4, sc1=sc1: e.reduce_max(out=sc1, in_=gsc4, axis=AX.X), RR, RR)
                    ts('dve', gm, gsc4, sc1, ALU.is_ge, RR, RR)
                    ok3 = ok16.rearrange('p (g e) -> p g e', e=4)
                    for e4 in range(4):
                        cp('dve', ok3[:, :, e4], gm, RR, RR)
                    ts('dve', mk, pr, 1.0, ALU.add, RR, RR)
                    tt('dve', mk, mk, ok16, ALU.mult, RR, RR)
                    ts('dve', mk, mk, -1.0, ALU.add, RR, RR)
                    S.op('dve', lambda e, mk=mk, sc1=sc1: e.reduce_max(out=sc1, in_=mk, axis=AX.X), RR, RR)
                    ts('dve', mk2, mk, sc1, ALU.is_ge, RR, RR)
                    stt('dve', mk2, mk2, -2.0, mk, ALU.mult, ALU.add, RR, RR)
                    S.op('dve', lambda e, mk2=mk2, sc2=sc2: e.reduce_max(out=sc2, in_=mk2, axis=AX.X), RR, RR)
                    tt('dve', sc3, sc1, sc2, ALU.add, RR, RR)
                    recip(sc4, sc3, RR, RR)
                    ts('dve', mk2, mk, sc2, ALU.is_ge, RR, RR)
                    tt('dve', mk2, mk2, pr, ALU.mult, RR, RR)
                    ts('dve', gts[:, sti, :], mk2, sc4, ALU.mult, RR + [R_gts[sti]], [R_gts[sti]])
                S.barrier()
                A.release()
                if stop == 'C':
                    S.emit(); print('sched stats', S.stats); return nc
                A.mark()
                weg = [A.alloc([8, 512], BF16) for _ in range(2)]; weu = [A.alloc([8, 512], BF16) for _ in range(2)]
                wed = [A.alloc([4, D], BF16) for _ in range(2)]; R_we = [Res() for _ in range(2)]
                sg = [A.alloc([512], F32) for _ in range(2)]; R_sg = [Res() for _ in range(2)]
                hT = [A.alloc([4, 512], BF16) for _ in range(2)]; R_hT = [Res() for _ in range(2)]
                hres = [R_hx2[i] for i in range(n // 128)]
                for ex in range(NE):
                    wb = ex % 2
                    if not first_blk:
                        dma('sp', weg[wb].rearrange('p a d -> p (a d)'), wc_g[ex], [R_wc[ex]], [R_we[wb]], key=KP_ld.next())
                        dma('sp', weu[wb].rearrange('p a d -> p (a d)'), wc_u[ex], [R_wc[ex]], [R_we[wb]], key=KP_ld.next())
                        dma('sp', wed[wb].rearrange('p a d -> p (a d)'), wc_d[ex], [R_wc[ex]], [R_we[wb]], key=KP_ld.next())
                    for hh in (range(2) if first_blk else []):
                        load_cast(weg[wb][:, hh * 4:(hh + 1) * 4, :],
                                  w_e_gate[l, ex, hh * 512:(hh + 1) * 512, :].rearrange('(k p) c -> p k c', p=128), R_we[wb])
                    for hh in (range(2) if first_blk else []):
                        load_cast(weu[wb][:, hh * 4:(hh + 1) * 4, :],
                                  w_e_up[l, ex, hh * 512:(hh + 1) * 512, :].rearrange('(k p) c -> p k c', p=128), R_we[wb])
                    for hh in (range(2) if first_blk else []):
                        load_cast(wed[wb][:, :, hh * 512:(hh + 1) * 512],
                                  w_e_down[l, ex, :, hh * 512:(hh + 1) * 512].rearrange('(k p) c -> p k c', p=128), R_we[wb])
                    if first_blk:
                        dma('pool', wc_g[ex], weg[wb].rearrange('p a d -> p (a d)'), [R_we[wb]], [R_wc[ex]], key=KP_st.next())
                        dma('pool', wc_u[ex], weu[wb].rearrange('p a d -> p (a d)'), [R_we[wb]], [R_wc[ex]], key=KP_st.next())
                        dma('pool', wc_d[ex], wed[wb].rearrange('p a d -> p (a d)'), [R_we[wb]], [R_wc[ex]], key=KP_st.next())
                    hb_ = ex % 2
                    for dc in range(4):
                        pg = 2 + (dc % 2) * 2
                        for k in range(8):
                            mm(PS[pg][:, 0:n], weg[wb][:, k, dc * 128:(dc + 1) * 128], hx2T[:, k, 0:n], k == 0, k == 7,
                               [R_we[wb]] + hres, [RPS[pg]])
                        for k in range(8):
                            mm(PS[pg + 1][:, 0:n], weu[wb][:, k, dc * 128:(dc + 1) * 128], hx2T[:, k, 0:n], k == 0, k == 7,
                               [R_we[wb]] + hres, [RPS[pg + 1]])
                        si = dc % 2
                        act(sg[si][:, 0:n], PS[pg][:, 0:n], AF.Silu, [RPS[pg]], [R_sg[si]])
                        tt('dve', hT[hb_][:, dc, 0:n], sg[si][:, 0:n], PS[pg + 1][:, 0:n], ALU.mult,
                           [R_sg[si], RPS[pg + 1]], [R_hT[hb_]])
                    for tl_ in range(n // 128):
                        sti = tl_
                        for hf in range(2):
                            pm = 6 + hf
                            for dc in range(4):
                                mm(PS[pm], hT[hb_][:, dc, tl_ * 128:(tl_ + 1) * 128], wed[wb][:, dc, hf * 512:(hf + 1) * 512],
                                   dc == 0, dc == 3, [R_hT[hb_], R_we[wb]], [RPS[pm]])
                            if ex == 0:
                                ts('dve', macc[:, sti, hf * 512:(hf + 1) * 512], PS[pm], gts[:, sti, ex:ex + 1], ALU.mult,
                                   [RPS[pm], R_gts[sti]], [R_macc[sti]])
                            else:
                                stt('dve', macc[:, sti, hf * 512:(hf + 1) * 512], PS[pm], gts[:, sti, ex:ex + 1],
                                    macc[:, sti, hf * 512:(hf + 1) * 512], ALU.mult, ALU.add,
                                    [RPS[pm], R_gts[sti]], [R_macc[sti]])
                for tl_ in range(n // 128):
                    tk0 = t0 + tl_ * 128
                    sti = tl_
                    xi = xi_i[0] % 2
                    xi_i[0] += 1
                    dma('sp', xin[xi], xs1[tk0:tk0 + 128, :], [R_xs1], [R_xin[xi]], key=KP_ld.next())
                    tt('dve', yb[xi], macc[:, sti, :], modbc[:, 2 + j, :], ALU.mult, [R_macc[sti], R_modbc], [R_yb[xi]])
                    stt('dve', yb[xi], xin[xi], ALPHA, yb[xi], ALU.mult, ALU.add, [R_xin[xi], R_yb[xi]], [R_yb[xi]])
                    if last:
                        layer_norm_tile(yb[xi], R_yb[xi], 2, out[tk0:tk0 + 128, :], R_out)
                    else:
                        layer_norm_tile(yb[xi], R_yb[xi], 2, xs2[tk0:tk0 + 128, :], R_out)
                S.barrier()
                A.release()
                if stop == 'D':
                    S.emit(); print('sched stats', S.stats); return nc
            S.barrier()
            A.release()
            A.release()
            A.release()
            A.release()
        S.emit()
    print('sched stats', S.stats, 'arena peak', A.peak)
    return nc


def _perm_cols():
    cols = []
    for base in (0, 512):
        for h in range(4):
            prim, sw = [], []
            for m in range(2):
                o = base + h * 128 + m * 64
                p_ = [o + 2 * i for i in range(32)] + [o + 2 * i + 1 for i in range(32)]
                s_ = [o + 2 * i + 1 for i in range(32)] + [o + 2 * i for i in range(32)]
                prim += p_
                sw += s_
            cols += prim + sw
    cols += list(range(1024, 1536))
    for c in range(4):
        cols += list(range(1536 + c * 128, 1536 + (c + 1) * 128))
        cols += list(range(2048 + c * 128, 2048 + (c + 1) * 128))
    cols += list(range(2560, 4608))
    return np.array(cols, dtype=np.int64)


def _rope_tables(half):
    t = np.arange(half * NLAT, (half + 1) * NLAT)
    row = (t // 64).astype(np.float32)
    col = (t % 64).astype(np.float32)
    inv_freq = (np.float32(10000.0) ** (-np.arange(0, 32, 2, dtype=np.float32) / np.float32(32))).astype(np.float32)
    ang = np.concatenate([row[:, None] * inv_freq, col[:, None] * inv_freq], -1).astype(np.float32)
    c = np.cos(ang).astype(np.float32).T
    s = np.sin(ang).astype(np.float32).T
    C = np.concatenate([c, c, c, c], 0)
    Sg = np.concatenate([-s, s, -s, s], 0)
    return np.ascontiguousarray(C), np.ascontiguousarray(Sg)


_NC_CACHE = {}


def kernel(x, c, ctx, c_ctx, w_ada, b_ada, w_in, lam_q1, lam_k1, lam_q2, lam_k2, subln_g,
           w_attn_o, conv_w, conv_b, conv_ln_g, conv_ln_b, w_conv_o, w_out, ln1_g, ln1_b,
           w_router, b_router, w_e_gate, w_e_up, w_e_down, ln2_g, ln2_b, _dbg=False, _stop=None):
    f = lambda a: np.ascontiguousarray(np.asarray(a, dtype=np.float32))
    x, c, ctx, c_ctx = f(x), f(c), f(ctx), f(c_ctx)
    key = (bool(_dbg), _stop)
    if key not in _NC_CACHE:
        _NC_CACHE[key] = build_program(dbg=_dbg, stop=_stop)
    nc = _NC_CACHE[key]
    cols = _perm_cols()
    w_in_r = np.ascontiguousarray(f(w_in)[:, :, cols])
    b_adaT = np.ascontiguousarray(f(b_ada).reshape(2, 48, 128).transpose(2, 0, 1).reshape(128, 96))
    lamv = np.ascontiguousarray(np.stack([f(lam_q1), f(lam_k1), f(lam_q2), f(lam_k2)], 1).reshape(2, 256))
    convw = np.ascontiguousarray(f(conv_w).reshape(2, 31, 4, 128).transpose(3, 0, 2, 1).reshape(128, 248))
    convv = np.ascontiguousarray(np.stack([f(conv_b), f(conv_ln_g), f(conv_ln_b)], 1).reshape(2, 3, 4, 128)
                                 .transpose(3, 0, 1, 2).reshape(128, 24))
    lnv = np.ascontiguousarray(np.stack([f(ln1_g), f(ln1_b), f(ln2_g), f(ln2_b)], 1).reshape(2, 4 * D))
    w_router_r = np.ascontiguousarray(f(w_router).reshape(8, 128, 16).transpose(1, 0, 2).reshape(128, 128))
    shared = dict(identd=np.eye(128, dtype=np.float32), w_ada=f(w_ada), b_adaT=b_adaT, w_in_r=w_in_r, lamv=lamv,
                  sublng=f(subln_g), w_attn_o=f(w_attn_o), w_conv_o=f(w_conv_o), w_out=f(w_out), convw=convw,
                  convv=convv, lnv=lnv, w_router_r=w_router_r, b_router=f(b_router).reshape(1, 16),
                  w_e_gate=f(w_e_gate), w_e_up=f(w_e_up), w_e_down=f(w_e_down))
    if _stop in ('p0', 'A', 'X', 'B', 'C'):
        for kk in ('w_e_gate', 'w_e_up', 'w_e_down'):
            shared[kk] = np.ascontiguousarray(shared[kk][:, 0:1])
    ropes = [_rope_tables(h) for h in range(2)]
    in_maps = []
    for core in range(8):
        b, half = core // 2, core % 2
        cc = np.stack([c[b], c_ctx], 1)
        ccT = np.ascontiguousarray(cc.reshape(8, 128, 2).transpose(1, 0, 2).reshape(128, 16))
        hs = np.zeros((128, 2), np.float32)
        hs[:, 0] = 1.0 if half == 1 else 0.0
        hs[:, 1] = 1.0 if half == 0 else 0.0
        m = dict(shared)
        m.update(x_own=np.ascontiguousarray(x[b, half * NLAT:(half + 1) * NLAT]), ctxb=np.ascontiguousarray(ctx[b]),
                 ccT=ccT, ropeC=ropes[half][0], ropeS=ropes[half][1], halo_sel=hs)
        in_maps.append(m)
    res = run_bass_kernel_spmd(nc, in_maps, core_ids=list(range(8)))
    outp = np.empty((4, 8192, D), np.float32)
    for core in range(8):
        b, half = core // 2, core % 2
        outp[b, half * NLAT:(half + 1) * NLAT] = res.results[core]['out']
    if _dbg:
        return outp, res
    return outp
```

```python
import bisect
import contextlib
import math
import numpy as np
import concourse.bass as bass
import concourse.mybir as mybir
from concourse.bass_utils import run_bass_kernel_spmd

F32 = mybir.dt.float32
BF16 = mybir.dt.bfloat16
U8 = mybir.dt.uint8
AF = mybir.ActivationFunctionType
ALU = mybir.AluOpType
AX = mybir.AxisListType

ENGS = ('pe', 'act', 'dve', 'pool', 'sp')

D = 1024
NLAT = 4096
NCTX = 256
TOK = NLAT + NCTX
NE = 16
DE = 512
INW = 5632
ALPHA = 4.0 ** 0.25
EPS = 1e-5
BROWS = 8256
KROWS = 4096
VROWS = 4128


class Res:
    __slots__ = ('w', 'r', 'name')

    def __init__(self, name=''):
        self.w = {}
        self.r = {}
        self.name = name


class Sched:
    def __init__(self, nc):
        self.nc = nc
        self.progs = {e: [] for e in ENGS}
        self.cnt = {e: 0 for e in ENGS}
        self.known = {e: {} for e in ENGS}
        self.snaps = {e: ([0], [{}]) for e in ENGS}
        self.dma_keys = []
        self.cc_keys = set()
        self.targets = {e: set() for e in ENGS}

    def dma_key(self, name, cc=False):
        k = 'd_' + name
        assert k not in self.cnt
        self.cnt[k] = 0
        self.dma_keys.append(k)
        if cc:
            self.cc_keys.add(k)
        return k

    def _merge_known(self, eng, key, val):
        kn = self.known[eng]
        if kn.get(key, 0) < val:
            kn[key] = val
        if key in self.snaps:
            counts, dicts = self.snaps[key]
            i = bisect.bisect_right(counts, val) - 1
            for k2, v2 in dicts[i].items():
                if kn.get(k2, 0) < v2:
                    kn[k2] = v2

    def _add_waits(self, eng, need):
        kn = self.known[eng]
        waits = [(k, v) for k, v in need.items() if kn.get(k, 0) < v]
        for k, v in waits:
            self._merge_known(eng, k, v)
            if k in self.targets:
                self.targets[k].add(v)
        if waits:
            counts, dicts = self.snaps[eng]
            counts.append(self.cnt[eng] + 1)
            dicts.append(dict(kn))
        return waits

    def op(self, eng, fn, reads=(), writes=(), dma=None):
        need = {}

        def add(clk, same_ok):
            if clk is None:
                return
            k, v = clk
            if k == eng and (eng == 'pe' or not same_ok):
                return
            if need.get(k, 0) < v:
                need[k] = v
        for r in reads:
            for k, v in r.w.items():
                add((k, v), True)
        for w in writes:
            for k, v in w.w.items():
                add((k, v), True)
            for k, v in w.r.items():
                add((k, v), False)
        if dma is not None and self.cnt[dma] > 0:
            if need.get(dma, 0) < self.cnt[dma]:
                need[dma] = self.cnt[dma]
        waits = self._add_waits(eng, need)
        if dma is None:
            self.cnt[eng] += 1
            clk = (eng, self.cnt[eng])
        else:
            self.cnt[dma] += 1
            clk = (dma, self.cnt[dma])
        self.progs[eng].append((fn, waits, clk))
        for r in reads:
            if r.r.get(clk[0], 0) < clk[1]:
                r.r[clk[0]] = clk[1]
        for w in writes:
            if w.w.get(clk[0], 0) < clk[1]:
                w.w[clk[0]] = clk[1]
            w.r = {}
        return clk

    def barrier(self):
        tot = dict(self.cnt)
        for e in ENGS:
            need = {k: v for k, v in tot.items() if k != e and v > 0}
            waits = self._add_waits(e, need)
            if waits:
                self.progs[e].append((None, waits, None))

    def emit(self, final_engine='sp'):
        nc = self.nc
        tot = dict(self.cnt)
        need = {k: v for k, v in tot.items() if k != final_engine and v > 0}
        fw = self._add_waits(final_engine, need)
        if fw:
            self.progs[final_engine].append((None, fw, None))
        tl = {e: sorted(self.targets[e]) for e in ENGS}

        def semval(k, v):
            if k in tl:
                i = bisect.bisect_left(tl[k], v)
                assert i < len(tl[k]) and tl[k][i] == v, (k, v)
                return i + 1
            if k in self.cc_keys:
                return v
            return 16 * v
        keys = list(ENGS) + self.dma_keys
        with contextlib.ExitStack() as st:
            sems = {k: st.enter_context(nc.semaphore('s_' + k)) for k in keys}
            block = st.enter_context(nc.Block())
            handles = {'pe': block.tensor, 'act': block.scalar, 'dve': block.vector,
                       'pool': block.gpsimd, 'sp': block.sync}

            def mk(ename):
                prog = self.progs[ename]
                tset = self.targets[ename]

                def body(e):
                    for fn, waits, clk in prog:
                        for k, v in waits:
                            e.wait_ge(sems[k], semval(k, v))
                        if fn is None:
                            continue
                        ins = fn(e)
                        if clk[0] == ename:
                            if clk[1] in tset:
                                ins.then_inc(sems[ename], 1)
                        elif clk[0] in self.cc_keys:
                            ins.then_inc(sems[clk[0]])
                        else:
                            ins.then_inc(sems[clk[0]], 16)
                return body
            for ename in ENGS:
                handles[ename](mk(ename))
        self.stats = {e: (len(self.progs[e]), len(tl[e])) for e in ENGS}


class Arena:
    def __init__(self, ap_u8, size):
        self.t = ap_u8
        self.size = size
        self.off = 0
        self.marks = []
        self.peak = 0

    def alloc(self, shape_free, dtype, parts=128):
        esz = {F32: 4, BF16: 2}[dtype]
        n = int(np.prod(shape_free))
        nbytes = n * esz
        off = (self.off + 63) // 64 * 64
        assert off + nbytes <= self.size, f'SBUF arena overflow: {off}+{nbytes}>{self.size}'
        self.off = off + nbytes
        self.peak = max(self.peak, self.off)
        ap = self.t[0:parts, off:off + nbytes].bitcast(dtype)
        if len(shape_free) > 1:
            names = ' '.join(f'a{i}' for i in range(len(shape_free)))
            kw = {f'a{i}': int(s) for i, s in enumerate(shape_free)}
            ap = ap.rearrange(f'p ({names}) -> p {names}', **kw)
        return ap

    def mark(self):
        self.marks.append(self.off)

    def release(self):
        self.off = self.marks.pop()


ARENA_BYTES = 200 * 1024


def build_program(dbg=False, n_layers=2, stop=None):
    nc = bass.Bass("TRN2", target_bir_lowering=False)

    def din(name, shape, dt=F32):
        return nc.dram_tensor(name, list(shape), dt, kind="ExternalInput").ap()

    x_own = din("x_own", [NLAT, D])
    ctxb = din("ctxb", [NCTX, D])
    ccT = din("ccT", [128, 16])
    identd = din("identd", [128, 128])
    w_ada = din("w_ada", [2, D, 6 * D])
    b_adaT = din("b_adaT", [128, 96])
    w_in_r = din("w_in_r", [2, D, INW])
    ropeC = din("ropeC", [128, NLAT])
    ropeS = din("ropeS", [128, NLAT])
    lamv = din("lamv", [2, 256])
    sublng = din("sublng", [2, 128])
    w_attn_o = din("w_attn_o", [2, 512, D])
    w_conv_o = din("w_conv_o", [2, 512, D])
    w_out = din("w_out", [2, D, D])
    convw = din("convw", [128, 2 * 4 * 31])
    convv = din("convv", [128, 2 * 3 * 4])
    lnv = din("lnv", [2, 4 * D])
    w_router_r = din("w_router_r", [128, 8 * 16])
    b_router = din("b_router", [1, 16])
    halo_sel = din("halo_sel", [128, 2])
    nexp = 1 if stop in ('p0', 'A', 'X', 'B', 'C') else NE
    w_e_gate = din("w_e_gate", [2, nexp, D, DE])
    w_e_up = din("w_e_up", [2, nexp, D, DE])
    w_e_down = din("w_e_down", [2, nexp, DE, D])
    out = nc.dram_tensor("out", [NLAT, D], F32, kind="ExternalOutput").ap()
    kind_dbg = "ExternalOutput" if dbg else "Internal"
    xs1 = nc.dram_tensor("xs1", [TOK, D], F32, kind=kind_dbg).ap()
    xs2 = nc.dram_tensor("xs2", [TOK, D], F32, kind=kind_dbg).ap()
    gsc = nc.dram_tensor("gsc", [128, 16, TOK], BF16).ap()
    qsc = nc.dram_tensor("qsc", [4, 128, NLAT], BF16).ap()
    ysc = nc.dram_tensor("ysc", [128, 4, NLAT + 32], BF16).ap()
    ycsc = nc.dram_tensor("ycsc", [128, 4, NCTX + 32], BF16).ap()
    ansc = nc.dram_tensor("ansc", [128, 4, TOK], BF16).ap()
    wc_g = nc.dram_tensor("wc_g", [NE, 128, 8 * 512], BF16).ap()
    wc_u = nc.dram_tensor("wc_u", [NE, 128, 8 * 512], BF16).ap()
    wc_d = nc.dram_tensor("wc_d", [NE, 128, 4 * D], BF16).ap()
    wc_3 = nc.dram_tensor("wc_3", [128, 16 * D], BF16).ap()
    CH_ROWS = [1024] * 8 + [32]
    bounce_t = [[nc.dram_tensor(f"bounce{l}_{c}", [CH_ROWS[c], 512], BF16) for c in range(9)] for l in range(2)]
    gath_t = [[nc.dram_tensor(f"gath{l}_{c}", [2 * CH_ROWS[c], 512], BF16) for c in range(9)] for l in range(2)]

    S = Sched(nc)
    st = contextlib.ExitStack()
    with st:
        arena_t = st.enter_context(nc.sbuf_tensor("arena", [128, ARENA_BYTES], U8))
        A = Arena(arena_t.ap() if hasattr(arena_t, 'ap') else arena_t[:, :], ARENA_BYTES)
        PSh = [st.enter_context(nc.psum_tensor(f"ps{i}", [128, 512], F32)) for i in range(4)]
        SCh = [st.enter_context(nc.psum_tensor(f"sc{i}", [128, 1024], F32)) for i in range(2)]
        PS = [p[:, :] for p in PSh]
        SC = [p[:, :] for p in SCh]
        for sc_ in SC:
            PS.append(sc_[:, 0:512])
            PS.append(sc_[:, 512:1024])
        RPS = [Res(f'ps{i}') for i in range(8)]

        def mm(o, lhsT, rhs, start, stop, reads, writes, tp=None, skip=False):
            if tp is None:
                S.op('pe', lambda e: e.matmul(o, lhsT=lhsT, rhs=rhs, start=start, stop=stop,
                                              skip_group_check=skip), reads, writes)
            else:
                S.op('pe', lambda e: e.matmul(o, lhsT=lhsT, rhs=rhs, start=start, stop=stop,
                                              skip_group_check=skip, tile_position=tp), reads, writes)

        def tr(o, in_, reads, writes):
            S.op('pe', lambda e: e.transpose(out=o, in_=in_, identity=identf), reads + [R_const], writes)

        def act(o, in_, func, reads, writes, bias=None, scale=None, accum=None):
            kw = {}
            if bias is not None:
                kw['bias'] = bias
            if scale is not None:
                kw['scale'] = scale
            if accum is not None:
                kw['accum_out'] = accum
            S.op('act', lambda e: e.activation(out=o, in_=in_, func=func, **kw), reads, writes)

        def tt(eng, o, a, b, op, reads, writes):
            S.op(eng, lambda e: e.tensor_tensor(out=o, in0=a, in1=b, op=op), reads, writes)

        def ts(eng, o, a, s1, op0, reads, writes, s2=None, op1=None):
            if op1 is None:
                S.op(eng, lambda e: e.tensor_scalar(out=o, in0=a, scalar1=s1, scalar2=None, op0=op0), reads, writes)
            else:
                S.op(eng, lambda e: e.tensor_scalar(out=o, in0=a, scalar1=s1, scalar2=s2, op0=op0, op1=op1),
                     reads, writes)

        def stt(eng, o, a, s, b, op0, op1, reads, writes):
            S.op(eng, lambda e: e.scalar_tensor_tensor(out=o, in0=a, scalar=s, in1=b, op0=op0, op1=op1),
                 reads, writes)

        def cp(eng, o, a, reads, writes):
            if eng == 'act':
                S.op(eng, lambda e: e.activation(out=o, in_=a, func=AF.Copy), reads, writes)
            else:
                S.op(eng, lambda e: e.tensor_copy(out=o, in_=a), reads, writes)

        def memset(eng, o, v, writes):
            S.op(eng, lambda e: e.memset(o, v), [], writes)

        def recip(o, a, reads, writes):
            S.op('dve', lambda e: e.reciprocal(out=o, in_=a), reads, writes)

        dma_ctr = [0]

        def dma(eng, o, i, reads, writes, key=None):
            if key is None:
                key = KP_misc.get(eng)
            elif isinstance(key, KeyPool):
                key = key.get(eng)
            S.op(eng, lambda e: e.dma_start(out=o, in_=i), reads, writes, dma=key)

        class KeyPool:
            def __init__(self, name, n):
                self.name = name
                self.n = n
                self.keys = {}
                self.i = {}

            def next(self):
                return self

            def get(self, eng):
                if eng not in self.keys:
                    self.keys[eng] = [S.dma_key(f'{self.name}_{eng}{i}') for i in range(self.n)]
                    self.i[eng] = 0
                k = self.keys[eng][self.i[eng] % self.n]
                self.i[eng] += 1
                return k
        KP_ld = KeyPool('ld', 20)
        KP_st = KeyPool('st', 12)
        KP_misc = KeyPool('misc', 4)

        identf = A.alloc([128], F32)
        onesf = A.alloc([128], F32)
        onesm = A.alloc([128], F32)
        epsc = A.alloc([1], F32)
        silucc = A.alloc([16], F32)
        wr_sb = A.alloc([8, 16], F32)
        brt_bc = A.alloc([16], F32)
        hsel = A.alloc([2], F32)
        R_const = Res('const')
        dma('sp', identf, identd, [], [R_const])
        memset('pool', onesf, 1.0, [R_const])
        memset('pool', onesm, 1.0 / 512.0, [R_const])
        memset('pool', epsc, EPS, [R_const])
        dma('sp', silucc, ccT, [], [R_const])
        dma('sp', wr_sb.rearrange('p k e -> p (k e)'), w_router_r, [], [R_const])
        dma('sp', brt_bc, b_router.partition_broadcast(128), [], [R_const])
        dma('sp', hsel, halo_sel, [], [R_const])
        act(silucc, silucc, AF.Silu, [R_const], [R_const])

        modT = A.alloc([48, 2], F32); R_mod = Res('mod')
        modbc = A.alloc([4, D], F32); R_modbc = Res('modbc')
        lnbc = A.alloc([4, D], F32); R_lnbc = Res('lnbc')
        neglam = A.alloc([1], F32); R_lam = Res('lam')
        gsub = A.alloc([128], F32); R_gsub = Res('gsub')
        cw = A.alloc([4, 31], F32); cvv = A.alloc([3, 4], F32); R_cw = Res('cw')
        kcT = A.alloc([4, NCTX], BF16); R_kcT = Res('kcT')
        vcs = A.alloc([2, 4, 129], BF16); R_vcs = Res('vcs')
        qcT = A.alloc([4, NCTX], BF16); R_qcT = Res('qcT')

        blocks_lat = [(512 * b, 512) for b in range(8)]
        blk_ctx = (NLAT, NCTX)

        def x_in_ap(l, t0, n):
            if l == 0:
                if t0 < NLAT:
                    return x_own[t0:t0 + n, :]
                return ctxb[t0 - NLAT:t0 - NLAT + n, :]
            return xs2[t0:t0 + n, :]

        for l in range(n_layers):
            last = (l == 1)
            lam_init = 0.8 - 0.6 * math.exp(-0.3 * l)
            bounce = bounce_t[l]
            gath = gath_t[l]
            S.barrier()
            A.mark()
            A.mark()
            stage = [A.alloc([6 * D], F32) for _ in range(2)]
            R_stage = [Res() for _ in range(2)]
            modacc = A.alloc([96], F32); R_modacc = Res()
            badat = A.alloc([96], F32); R_badat = Res()
            lamt = A.alloc([256], F32); R_lamt = Res()
            lamp = A.alloc([128], F32)
            lams = A.alloc([4], F32)
            tmpb = A.alloc([128], F32); R_tmpb = Res()
            dma('sp', badat, b_adaT, [], [R_badat])
            dma('sp', lamt, lamv[l:l + 1, :].partition_broadcast(128), [], [R_lamt])
            dma('sp', gsub, sublng[l:l + 1, :].partition_broadcast(128), [], [R_gsub])
            dma('sp', lnbc.rearrange('p a d -> p (a d)'), lnv[l:l + 1, :].partition_broadcast(128), [], [R_lnbc])
            dma('sp', cw.rearrange('p c k -> p (c k)'), convw[:, l * 124:(l + 1) * 124], [], [R_cw])
            dma('sp', cvv.rearrange('p a c -> p (a c)'), convv[:, l * 12:(l + 1) * 12], [], [R_cw])
            for k in range(8):
                sb = k % 2
                dma('sp' if k % 2 == 0 else 'pool', stage[sb], w_ada[l, k * 128:(k + 1) * 128, :], [], [R_stage[sb]],
                    key=KP_ld.next())
                for ch in range(48):
                    mm(PS[0][:, 2 * ch:2 * ch + 2], stage[sb][:, ch * 128:(ch + 1) * 128], silucc[:, 2 * k:2 * k + 2],
                       True, True, [R_stage[sb], R_const], [RPS[0]])
                if k == 0:
                    cp('dve', modacc, PS[0][:, 0:96], [RPS[0]], [R_modacc])
                else:
                    tt('dve', modacc, modacc, PS[0][:, 0:96], ALU.add, [RPS[0], R_modacc], [R_modacc])
            macc3 = modacc.rearrange('p (c j) -> p c j', j=2)
            for j in range(2):
                tt('dve', modT[:, :, j], macc3[:, :, j], badat[:, l * 48:(l + 1) * 48], ALU.add,
                   [R_modacc, R_badat], [R_mod])
            for mi in (1, 4):
                ts('dve', modT[:, mi * 8:(mi + 1) * 8, :], modT[:, mi * 8:(mi + 1) * 8, :], 1.0, ALU.add, [R_mod], [R_mod])
            for bi, (mi, j) in enumerate([(2, 0), (2, 1), (5, 0), (5, 1)]):
                for kh in range(2):
                    for k4 in range(4):
                        k = kh * 4 + k4
                        ts('dve', tmpb, onesf, modT[:, mi * 8 + k, j:j + 1], ALU.mult, [R_mod, R_const, R_tmpb], [R_tmpb])
                        mm(PS[1][:, k4 * 128:(k4 + 1) * 128], tmpb, identf, True, True, [R_tmpb, R_const], [RPS[1]])
                    cp('dve', modbc[:, bi, kh * 512:(kh + 1) * 512], PS[1], [RPS[1]], [R_modbc])
            for i in range(2):
                tt('dve', lamp[:, 0:64], lamt[:, 128 * i:128 * i + 64], lamt[:, 128 * i + 64:128 * i + 128], ALU.mult,
                   [R_lamt, R_lam], [R_lam])
                S.op('dve', lambda e, i=i: e.reduce_sum(out=lams[:, i:i + 1], in_=lamp[:, 0:64], axis=AX.X),
                     [R_lam], [R_lam])
            act(lams[:, 0:2], lams[:, 0:2], AF.Exp, [R_lam], [R_lam])
            tt('dve', lams[:, 2:3], lams[:, 1:2], lams[:, 0:1], ALU.subtract, [R_lam], [R_lam])
            ts('dve', neglam, lams[:, 2:3], -lam_init, ALU.add, [R_lam], [R_lam])
            ts('dve', gsub, gsub, 1.0 - lam_init, ALU.mult, [R_gsub], [R_gsub])
            S.barrier()
            A.release()
            if stop == 'p0':
                S.emit(); print('sched stats', S.stats); return nc

            A.mark()
            R_qT = Res('qsc'); R_yT = Res('ysc'); R_ycT = Res('ycsc')
            A.mark()
            zt = A.alloc([4, 16], BF16); R_zt = Res()
            hxT = A.alloc([8, TOK], BF16)
            R_hx = [Res(f'hx{t}') for t in range(34)]
            xt_b = [A.alloc([D], F32) for _ in range(2)]; R_xt = [Res() for _ in range(2)]
            wst = [A.alloc([8, 256], F32) for _ in range(2)]; R_wst = [Res() for _ in range(2)]
            wbf = [A.alloc([8, 256], BF16) for _ in range(2)]; R_wbf = [Res() for _ in range(2)]
            rCall = A.alloc([NLAT], F32); rSall = A.alloc([NLAT], F32)
            R_ropeall = Res()
            dma('sp', rCall, ropeC, [], [R_ropeall], key=KP_ld.next())
            dma('pool', rSall, ropeS, [], [R_ropeall], key=KP_ld.next())
            t1 = [A.alloc([512], F32) for _ in range(2)]; R_t1 = [Res() for _ in range(2)]
            t2 = [A.alloc([512], F32) for _ in range(2)]; R_t2 = [Res() for _ in range(2)]
            ot = [A.alloc([2, 512], BF16) for _ in range(3)]; R_ot = [Res() for _ in range(3)]
            vt = [A.alloc([2, 128], BF16) for _ in range(3)]; R_vt = [Res() for _ in range(3)]
            memset('pool', zt, 0.0, [R_zt])
            dma('pool', ycsc[:, :, 0:16], zt, [R_zt], [R_ycT])
            dma('pool', ycsc[:, :, NCTX + 16:NCTX + 32], zt, [R_zt], [R_ycT])
            for t in range(34):
                j = 0 if t < 32 else 1
                xb = t % 2
                dma('sp', xt_b[xb], x_in_ap(l, t * 128, 128), [], [R_xt[xb]], key=KP_ld.next())
                for kh in range(2):
                    pb = 2 + (2 * t + kh) % 2
                    for k4 in range(4):
                        k = kh * 4 + k4
                        tr(PS[pb][:, k4 * 128:(k4 + 1) * 128], xt_b[xb][:, k * 128:(k + 1) * 128], [R_xt[xb]], [RPS[pb]])
                    for k4 in range(4):
                        k = kh * 4 + k4
                        act(hxT[:, k, t * 128:(t + 1) * 128], PS[pb][:, k4 * 128:(k4 + 1) * 128], AF.Identity,
                            [RPS[pb], R_mod], [R_hx[t]], bias=modT[:, 0 * 8 + k, j:j + 1], scale=modT[:, 1 * 8 + k, j:j + 1])
            kreg = [bounce[h][:, :].rearrange('(p a) b -> p (a b)', p=128, a=8) for h in range(4)]
            ereg = bounce[8][:, :].rearrange('a b -> (a b)').rearrange(
                '(p c s t) -> p c s t', p=128, c=4, s=2, t=16)
            R_bounce = Res('bounce')
            R_gsc = Res('gsc')
            groups = ([('Q', h) for h in range(4)] + [('K', h) for h in range(4)] + [('V', g) for g in range(2)]
                      + [('U', c) for c in range(4)] + [('G', g) for g in range(8)])
            tok_blocks = blocks_lat + [blk_ctx]
            oti = 0
            vti = 0
            pbi = 0
            for gi, (kind, idx) in enumerate(groups):
                wb = gi % 2
                c0 = gi * 256
                dma('sp' if gi % 2 == 0 else 'pool', wst[wb],
                    w_in_r[l, :, c0:c0 + 256].rearrange('(k p) c -> p k c', p=128), [], [R_wst[wb]], key=KP_ld.next())
                cp('pool', wbf[wb], wst[wb], [R_wst[wb]], [R_wbf[wb]])
                for bi_, (t0, n) in enumerate(tok_blocks):
                    isctx = t0 >= NLAT
                    if isctx and last and kind in ('Q', 'U', 'G'):
                        continue
                    hx_res = [R_hx[t0 // 128 + i] for i in range(n // 128)]
                    if kind in ('Q', 'K', 'U', 'G'):
                        pa, pbk = 4 + (pbi % 2) * 2, 5 + (pbi % 2) * 2
                        pbi += 1
                        need_b = not (isctx and kind in ('Q', 'K'))
                        for half, pidx in ((0, pa), (1, pbk)):
                            if half == 1 and not need_b:
                                continue
                            for k in range(8):
                                mm(PS[pidx][:, 0:n], wbf[wb][:, k, half * 128:(half + 1) * 128], hxT[:, k, t0:t0 + n],
                                   k == 0, k == 7, [R_wbf[wb]] + hx_res, [RPS[pidx]])
                        if kind in ('Q', 'K'):
                            if isctx:
                                dst, rdst = (qcT, R_qcT) if kind == 'Q' else (kcT, R_kcT)
                                cp('dve', dst[:, idx, :], PS[pa][:, 0:n], [RPS[pa]], [rdst])
                            else:
                                tb = bi_ % 2
                                tt('dve', t1[tb], PS[pa], rCall[:, t0:t0 + 512], ALU.mult, [RPS[pa], R_ropeall], [R_t1[tb]])
                                tt('dve', t2[tb], PS[pbk], rSall[:, t0:t0 + 512], ALU.mult, [RPS[pbk], R_ropeall], [R_t2[tb]])
                                o = oti % 3
                                oti += 1
                                tt('pool', ot[o][:, 0, :], t1[tb], t2[tb], ALU.add, [R_t1[tb], R_t2[tb]], [R_ot[o]])
                                if kind == 'Q':
                                    dma('pool', qsc[idx, :, t0:t0 + 512], ot[o][:, 0, :], [R_ot[o]], [R_qT],
                                        key=KP_st.next())
                                else:
                                    dma('pool', kreg[idx][:, t0:t0 + 512], ot[o][:, 0, :], [R_ot[o]], [R_bounce],
                                        key=KP_st.next())
                        elif kind == 'U':
                            tb = bi_ % 2
                            act(t1[tb][:, 0:n], PS[pbk][:, 0:n], AF.Sigmoid, [RPS[pbk]], [R_t1[tb]])
                            o = oti % 3
                            oti += 1
                            tt('dve', ot[o][:, 0, 0:n], PS[pa][:, 0:n], t1[tb][:, 0:n], ALU.mult,
                               [RPS[pa], R_t1[tb]], [R_ot[o]])
                            if isctx:
                                dma('pool', ycsc[:, idx, 16:16 + n], ot[o][:, 0, 0:n], [R_ot[o]], [R_ycT], key=KP_st.next())
                            else:
                                dma('pool', ysc[:, idx, 16 + t0:16 + t0 + n], ot[o][:, 0, 0:n], [R_ot[o]], [R_yT],
                                    key=KP_st.next())
                        else:
                            o = oti % 3
                            oti += 1
                            act(ot[o][:, 0, 0:n], PS[pa][:, 0:n], AF.Sigmoid, [RPS[pa]], [R_ot[o]])
                            act(ot[o][:, 1, 0:n], PS[pbk][:, 0:n], AF.Sigmoid, [RPS[pbk]], [R_ot[o]])
                            dma('pool', gsc[:, 2 * idx:2 * idx + 2, t0:t0 + n], ot[o][:, :, 0:n], [R_ot[o]], [R_gsc],
                                key=KP_st.next())
                    else:
                        for tl_ in range(n // 128):
                            tk0 = t0 + tl_ * 128
                            pv = 4 + (pbi % 4)
                            pbi += 1
                            for k in range(8):
                                mm(PS[pv][:, 0:256], hxT[:, k, tk0:tk0 + 128], wbf[wb][:, k, :], k == 0, k == 7,
                                   [R_wbf[wb], R_hx[tk0 // 128]], [RPS[pv]])
                            if isctx:
                                cp('dve', vcs[:, tl_, 2 * idx:2 * idx + 2, 0:128],
                                   PS[pv][:, 0:256].rearrange('p (h e) -> p h e', h=2), [RPS[pv]], [R_vcs])
                            else:
                                v = vti % 3
                                vti += 1
                                cp('dve', vt[v][:, :, 0:128], PS[pv][:, 0:256].rearrange('p (h e) -> p h e', h=2),
                                   [RPS[pv]], [R_vt[v]])
                                dma('pool', bounce[4 + tk0 // 1024][tk0 % 1024:tk0 % 1024 + 128, idx * 256:(idx + 1) * 256],
                                    vt[v].rearrange('p h e -> p (h e)'), [R_vt[v]], [R_bounce], key=KP_st.next())
            memset('pool', vcs[:, :, :, 128:129], 1.0, [R_vcs])
            dma('pool', ereg[:, :, 0, :], ysc[:, :, 16:32], [R_yT], [R_bounce], key=KP_st.next())
            dma('pool', ereg[:, :, 1, :], ysc[:, :, NLAT:NLAT + 16], [R_yT], [R_bounce], key=KP_st.next())
            if stop == 'A':
                S.emit(); print('sched stats', S.stats); return nc
            R_gc = [Res(f'gath{c}') for c in range(9)]
            for c in range(9):
                cckey = S.dma_key(f'cc{l}_{c}', cc=True)
                S.op('pool', lambda e, b_=bounce[c], g_=gath[c]: e.collective_compute(
                    "AllGather", ALU.bypass, replica_groups=[[0, 1], [2, 3], [4, 5], [6, 7]],
                    ins=[b_.ap().opt()], outs=[g_.ap().opt()]), [R_bounce], [R_gc[c]], dma=cckey)
            S.barrier()
            A.release()
            A.mark()
            egs = [gath[8][r * 32:(r + 1) * 32, :].rearrange('a b -> (a b)').rearrange(
                '(p c s t) -> p c s t', p=128, c=4, s=2, t=16) for r in range(2)]
            dma('sp', ysc[:, :, 0:16], egs[0][:, :, 1, :], [R_gc[8]], [R_yT])
            dma('sp', ysc[:, :, NLAT + 16:NLAT + 32], egs[1][:, :, 0, :], [R_gc[8]], [R_yT])

            if stop == 'X':
                S.emit(); print('sched stats', S.stats); return nc
            R_anT = Res('ansc')
            A.mark()
            anst = [A.alloc([512], BF16) for _ in range(2)]; R_anst = [Res() for _ in range(2)]
            qblk = [A.alloc([512], BF16) for _ in range(2)]; R_qblk = [Res() for _ in range(2)]
            qbi = 0
            kT = [A.alloc([2 * NLAT], BF16) for _ in range(2)]; R_kT = [Res() for _ in range(2)]
            vh = [A.alloc([64, 129], BF16) for _ in range(2)]; R_vh = [Res() for _ in range(2)]
            pT = [A.alloc([1024], BF16) for _ in range(3)]; R_pT = [Res() for _ in range(3)]
            for i in range(2):
                memset('pool', vh[i][:, :, 128:129], 1.0, [R_vh[i]])
            nrm = A.alloc([8], F32); R_nrm = Res()
            tO = A.alloc([128], F32); oO = A.alloc([128], F32); onf = A.alloc([128], F32); R_o = Res()
            junk = A.alloc([128], F32)
            pti = 0
            sci = 0
            pst = [A.alloc([4, 512], F32) for _ in range(3)]; R_pst = [Res() for _ in range(3)]
            pbf = [A.alloc([4, 512], BF16) for _ in range(2)]; R_pbf = [Res() for _ in range(2)]
            R_wc3 = Res('wc3')
            R_wc = [Res(f'wc{e_}') for e_ in range(NE)]
            wc_3v = wc_3.rearrange('p (a d) -> p a d', a=16)
            pieces = []
            for hh in range(2):
                pieces.append((w_attn_o[l, :, hh * 512:(hh + 1) * 512].rearrange('(k p) c -> p k c', p=128),
                               wc_3v[:, 0:4, hh * 512:(hh + 1) * 512], R_wc3))
                pieces.append((w_conv_o[l, :, hh * 512:(hh + 1) * 512].rearrange('(k p) c -> p k c', p=128),
                               wc_3v[:, 4:8, hh * 512:(hh + 1) * 512], R_wc3))
                for kh in range(2):
                    pieces.append((w_out[l, kh * 512:(kh + 1) * 512, hh * 512:(hh + 1) * 512].rearrange('(k p) c -> p k c', p=128),
                                   wc_3v[:, 8 + kh * 4:8 + (kh + 1) * 4, hh * 512:(hh + 1) * 512], R_wc3))
            for ex in range(NE):
                wgv = wc_g[ex].rearrange('p (k c) -> p k c', k=8)
                wuv = wc_u[ex].rearrange('p (k c) -> p k c', k=8)
                wdv = wc_d[ex].rearrange('p (k c) -> p k c', k=4)
                for kh in range(2):
                    pieces.append((w_e_gate[l, ex, kh * 512:(kh + 1) * 512, :].rearrange('(k p) c -> p k c', p=128),
                                   wgv[:, kh * 4:(kh + 1) * 4, :], R_wc[ex]))
                    pieces.append((w_e_up[l, ex, kh * 512:(kh + 1) * 512, :].rearrange('(k p) c -> p k c', p=128),
                                   wuv[:, kh * 4:(kh + 1) * 4, :], R_wc[ex]))
                for hh in range(2):
                    pieces.append((w_e_down[l, ex, :, hh * 512:(hh + 1) * 512].rearrange('(k p) c -> p k c', p=128),
                                   wdv[:, :, hh * 512:(hh + 1) * 512], R_wc[ex]))
            pci = [0]

            def emit_pieces(cnt):
                for _ in range(cnt):
                    if pci[0] >= len(pieces):
                        return
                    src, dst, rdst = pieces[pci[0]]
                    i3 = pci[0] % 3
                    i2 = pci[0] % 2
                    pci[0] += 1
                    dma('sp', pst[i3], src, [], [R_pst[i3]], key=KP_ld.next())
                    cp('pool' if pci[0] % 3 else 'dve', pbf[i2], pst[i3], [R_pst[i3]], [R_pbf[i2]])
                    dma('pool', dst, pbf[i2], [R_pbf[i2]], [rdst], key=KP_st.next())
            for h in range(4):
                hb = h % 2
                for r in range(2):
                    kg = gath[h][r * 1024:(r + 1) * 1024, :].rearrange('(p a) b -> p (a b)', p=128, a=8)
                    dma('sp', kT[hb][:, r * NLAT:(r + 1) * NLAT], kg, [R_gc[h]], [R_kT[hb]], key=KP_ld.next())
                    for q4 in range(4):
                        vg = gath[4 + q4][r * 1024:(r + 1) * 1024, h * 128:(h + 1) * 128].rearrange('(t p) e -> p t e', p=128)
                        dma('pool', vh[hb][:, r * 32 + q4 * 8:r * 32 + q4 * 8 + 8, 0:128],
                            vg, [R_gc[4 + q4]], [R_vh[hb]], key=KP_ld.next())
                qblocks = [(t0, n, False) for (t0, n) in blocks_lat]
                if not last:
                    qblocks.append((NLAT, NCTX, True))
                for (t0, n, isctx) in qblocks:
                    nqt = n // 128
                    qb_ = qbi % 2
                    qbi += 1
                    if not isctx:
                        dma('sp', qblk[qb_], qsc[h, :, t0:t0 + n], [R_qT], [R_qblk[qb_]], key=KP_ld.next())
                    ktiles = [('c', i) for i in range(2)]
                    if not isctx:
                        ktiles += [('l', i) for i in range(64)]
                    def emit_qk(kti):
                        kk, ki = ktiles[kti]
                        sb0 = 4 + (kti % 2) * 2
                        for m in range(2):
                            if kk == 'c':
                                lh = kcT[64 * m:64 * m + 64, h, ki * 128:(ki + 1) * 128]
                                rr = [R_kcT]
                            else:
                                lh = kT[hb][64 * m:64 * m + 64, ki * 128:(ki + 1) * 128]
                                rr = [R_kT[hb]]
                            if isctx:
                                rh = qcT[64 * m:64 * m + 64, h, :]
                                rr = rr + [R_qcT]
                            else:
                                rh = qblk[qb_][64 * m:64 * m + 64, 0:n]
                                rr = rr + [R_qblk[qb_]]
                            mm(PS[sb0 + m][:, 0:n], lh, rh, True, True, rr, [RPS[sb0 + m]], tp=(64 * m, 0))
                    emit_qk(0)
                    for kti, (kk, ki) in enumerate(ktiles):
                        sb0 = 4 + (kti % 2) * 2
                        if kti + 1 < len(ktiles):
                            emit_qk(kti + 1)
                        p = pti % 3
                        pti += 1
                        act(pT[p].rearrange('p (m q) -> p m q', m=2)[:, :, 0:n],
                            SC[(sb0 - 4) // 2].rearrange('p (m q) -> p m q', m=2)[:, :, 0:n], AF.Exp,
                            [RPS[sb0], RPS[sb0 + 1]], [R_pT[p]], scale=0.125)
                        for qt in range(nqt):
                            for m in range(2):
                                if kk == 'c':
                                    vv = vcs[:, ki, h, :]
                                    rv = [R_vcs]
                                else:
                                    vv = vh[hb][:, ki, :]
                                    rv = [R_vh[hb]]
                                mm(PS[qt][:, m * 129:(m + 1) * 129], pT[p][:, m * 512 + qt * 128:m * 512 + qt * 128 + 128], vv,
                                   (kti == 0 and m == 0), kti == len(ktiles) - 1, [R_pT[p]] + rv, [RPS[qt]], skip=True)
                    for qt in range(nqt):
                        acc = PS[qt]
                        recip(nrm[:, 0:1], acc[:, 128:129], [RPS[qt], R_nrm], [R_nrm])
                        recip(nrm[:, 1:2], acc[:, 257:258], [RPS[qt], R_nrm], [R_nrm])
                        tt('dve', nrm[:, 2:3], nrm[:, 1:2], neglam, ALU.mult, [R_nrm, R_lam], [R_nrm])
                        ts('dve', tO, acc[:, 129:257], nrm[:, 2:3], ALU.mult, [RPS[qt], R_nrm, R_o], [R_o])
                        stt('dve', oO, acc[:, 0:128], nrm[:, 0:1], tO, ALU.mult, ALU.add, [RPS[qt], R_nrm, R_o], [R_o])
                        memset('dve', nrm[:, 3:4], 0.0, [R_nrm])
                        act(junk, oO, AF.Square, [R_o], [R_o, R_nrm], accum=nrm[:, 3:4])
                        act(nrm[:, 4:5], nrm[:, 3:4], AF.Sqrt, [R_nrm, R_const], [R_nrm], bias=epsc, scale=1.0 / 128.0)
                        recip(nrm[:, 5:6], nrm[:, 4:5], [R_nrm], [R_nrm])
                        stt('dve', onf, oO, nrm[:, 5:6], gsub, ALU.mult, ALU.mult, [R_o, R_nrm, R_gsub], [R_o])
                        tr(PS[4][:, qt * 128:(qt + 1) * 128], onf, [R_o], [RPS[4]])
                        cp('dve', anst[qb_][:, qt * 128:(qt + 1) * 128], PS[4][:, qt * 128:(qt + 1) * 128],
                           [RPS[4]], [R_anst[qb_]])
                    dma('sp', ansc[:, h, t0:t0 + n], anst[qb_][:, 0:n], [R_anst[qb_]], [R_anT], key=KP_st.next())
                    emit_pieces(4)
            emit_pieces(len(pieces))
            S.barrier()
            A.release()

            if stop == 'B':
                S.emit(); print('sched stats', S.stats); return nc
            A.mark()
            wst2 = [A.alloc([4, 512], F32) for _ in range(3)]; R_wst2 = [Res() for _ in range(3)]
            wsi = [0]

            def load_cast(dst_ap, src_ap, rdst):
                i = wsi[0] % 3
                wsi[0] += 1
                dma('sp', wst2[i], src_ap, [], [R_wst2[i]], key=KP_ld.next())
                if wsi[0] % 3 == 0:
                    cp('act', dst_ap, wst2[i], [R_wst2[i]], [rdst])
                else:
                    cp('pool', dst_ap, wst2[i], [R_wst2[i]], [rdst])
            SBT = 512
            NST = SBT // 128
            hx2T = A.alloc([8, SBT], BF16); R_hx2 = [Res() for _ in range(NST)]
            macc = A.alloc([NST, D], F32); R_macc = [Res() for _ in range(NST)]
            gts = A.alloc([NST, 16], F32); R_gts = [Res() for _ in range(NST)]
            xin = [A.alloc([D], F32) for _ in range(2)]; R_xin = [Res() for _ in range(2)]
            yb = [A.alloc([D], F32) for _ in range(2)]; R_yb = [Res() for _ in range(2)]
            stat = A.alloc([12], F32); mv = A.alloc([4], F32); R_mv = Res()
            hx2f = A.alloc([8, 128], F32); R_hx2f = Res()
            rt = A.alloc([16 * 8], F32); R_rt = Res()
            xi_i = [0]

            def layer_norm_tile(yt, ry, gi, dst_dram, rdst):
                for c2 in range(2):
                    S.op('dve', lambda e, c2=c2: e.bn_stats(out=stat[:, c2 * 6:(c2 + 1) * 6], in_=yt[:, c2 * 512:(c2 + 1) * 512]),
                         [ry], [R_mv])
                S.op('dve', lambda e: e.bn_aggr(out=mv[:, 0:2], in_=stat), [R_mv], [R_mv])
                act(mv[:, 2:3], mv[:, 1:2], AF.Sqrt, [R_mv, R_const], [R_mv], bias=epsc, scale=1.0)
                recip(mv[:, 3:4], mv[:, 2:3], [R_mv], [R_mv])
                ts('dve', yt, yt, mv[:, 0:1], ALU.subtract, [ry, R_mv], [ry], s2=mv[:, 3:4], op1=ALU.mult)
                tt('pool', yt, yt, lnbc[:, gi, :], ALU.mult, [ry, R_lnbc], [ry])
                tt('pool', yt, yt, lnbc[:, gi + 1, :], ALU.add, [ry, R_lnbc], [ry])
                dma('sp', dst_dram, yt, [ry], [rdst], key=KP_st.next())

            R_xs1 = Res('xs1')
            R_out = Res('out')
            all_blocks = list(blocks_lat) + ([blk_ctx] if not last else [])
            for (t0, n) in all_blocks:
                isctx = t0 >= NLAT
                j = 1 if isctx else 0
                A.mark()
                w3all = A.alloc([16, D], BF16)
                wao = w3all[:, 0:4, :]; wco = w3all[:, 4:8, :]; wo = w3all[:, 8:16, :]
                R_w3 = Res('w3')
                first_blk = False
                if not first_blk:
                    dma('sp', w3all.rearrange('p a d -> p (a d)'), wc_3, [R_wc3], [R_w3], key=KP_ld.next())
                for hh in (range(2) if first_blk else []):
                    load_cast(wao[:, :, hh * 512:(hh + 1) * 512],
                              w_attn_o[l, :, hh * 512:(hh + 1) * 512].rearrange('(k p) c -> p k c', p=128), R_w3)
                    load_cast(wco[:, :, hh * 512:(hh + 1) * 512],
                              w_conv_o[l, :, hh * 512:(hh + 1) * 512].rearrange('(k p) c -> p k c', p=128), R_w3)
                    for kh in range(2):
                        load_cast(wo[:, kh * 4:(kh + 1) * 4, hh * 512:(hh + 1) * 512],
                                  w_out[l, kh * 512:(kh + 1) * 512, hh * 512:(hh + 1) * 512].rearrange('(k p) c -> p k c', p=128),
                                  R_w3)
                if first_blk:
                    dma('pool', wc_3, w3all.rearrange('p a d -> p (a d)'), [R_w3], [R_wc3], key=KP_st.next())
                z = A.alloc([4, 512], F32); R_z = [Res() for _ in range(4)]
                zq = A.alloc([512], F32); R_zq = Res()
                mean = A.alloc([512], F32); rstd = A.alloc([512], F32); R_st = Res()
                snT = A.alloc([4, 512], BF16); R_snT = Res()
                gtl = [A.alloc([2, 512], BF16) for _ in range(2)]; R_gtl = [Res() for _ in range(2)]
                GT = A.alloc([8, 512], BF16); R_GT = Res()
                tg1 = A.alloc([512], F32); tg2 = A.alloc([512], F32); R_tg = Res()
                yblk = A.alloc([4, 512 + 32], BF16); R_yblk = Res()
                anblk = A.alloc([4, 512], BF16); R_anblk = Res()
                if isctx:
                    dma('sp', yblk[:, :, 0:n + 32], ycsc[:, :, 0:n + 32], [R_ycT], [R_yblk], key=KP_ld.next())
                else:
                    dma('sp', yblk[:, :, 0:n + 32], ysc[:, :, t0:t0 + n + 32], [R_yT], [R_yblk], key=KP_ld.next())
                    if t0 == 0:
                        ts('dve', yblk[:, :, 0:16], yblk[:, :, 0:16], hsel[:, 0:1], ALU.mult, [R_yblk, R_const], [R_yblk])
                    if t0 + n == NLAT:
                        ts('dve', yblk[:, :, n + 16:n + 32], yblk[:, :, n + 16:n + 32], hsel[:, 1:2], ALU.mult,
                           [R_yblk, R_const], [R_yblk])
                dma('sp', anblk[:, :, 0:n], ansc[:, :, t0:t0 + n], [R_anT], [R_anblk], key=KP_ld.next())
                for c in range(4):
                    ts('dve', z[:, c, 0:n], yblk[:, c, 1:1 + n], cw[:, c, 0:1], ALU.mult,
                       [R_yblk, R_cw], [R_z[c]], s2=cvv[:, 0, c:c + 1], op1=ALU.add)
                    for k in range(1, 31):
                        stt('dve', z[:, c, 0:n], yblk[:, c, k + 1:k + 1 + n], cw[:, c, k:k + 1], z[:, c, 0:n],
                            ALU.mult, ALU.add, [R_yblk, R_cw], [R_z[c]])
                for c in range(4):
                    mm(PS[0][:, 0:n], onesm, z[:, c, 0:n], c == 0, c == 3, [R_const, R_z[c]], [RPS[0]])
                for c in range(4):
                    act(zq[:, 0:n], z[:, c, 0:n], AF.Square, [R_z[c]], [R_zq])
                    mm(PS[1][:, 0:n], onesm, zq[:, 0:n], c == 0, c == 3, [R_const, R_zq], [RPS[1]])
                cp('dve', mean[:, 0:n], PS[0][:, 0:n], [RPS[0]], [R_st])
                tt('dve', rstd[:, 0:n], mean[:, 0:n], mean[:, 0:n], ALU.mult, [R_st], [R_st])
                tt('dve', rstd[:, 0:n], PS[1][:, 0:n], rstd[:, 0:n], ALU.subtract, [RPS[1], R_st], [R_st])
                act(rstd[:, 0:n], rstd[:, 0:n], AF.Sqrt, [R_st, R_const], [R_st], bias=epsc, scale=1.0)
                recip(rstd[:, 0:n], rstd[:, 0:n], [R_st], [R_st])
                for c in range(4):
                    tt('dve', z[:, c, 0:n], z[:, c, 0:n], mean[:, 0:n], ALU.subtract, [R_st, R_z[c]], [R_z[c]])
                    tt('dve', z[:, c, 0:n], z[:, c, 0:n], rstd[:, 0:n], ALU.mult, [R_st, R_z[c]], [R_z[c]])
                    act(snT[:, c, 0:n], z[:, c, 0:n], AF.Silu, [R_z[c], R_cw], [R_snT],
                        bias=cvv[:, 2, c:c + 1], scale=cvv[:, 1, c:c + 1])
                for oc in range(8):
                    pa2 = 2 + (oc % 2) * 2
                    g_i = oc % 2
                    dma('sp', gtl[g_i][:, 0, 0:n], gsc[:, oc, t0:t0 + n], [R_gsc], [R_gtl[g_i]], key=KP_ld.next())
                    dma('sp', gtl[g_i][:, 1, 0:n], gsc[:, 8 + oc, t0:t0 + n], [R_gsc], [R_gtl[g_i]], key=KP_ld.next())
                    for hh in range(4):
                        mm(PS[pa2][:, 0:n], wao[:, hh, oc * 128:(oc + 1) * 128], anblk[:, hh, 0:n], hh == 0, hh == 3,
                           [R_w3, R_anblk], [RPS[pa2]])
                    for c in range(4):
                        mm(PS[pa2 + 1][:, 0:n], wco[:, c, oc * 128:(oc + 1) * 128], snT[:, c, 0:n], c == 0, c == 3,
                           [R_w3, R_snT], [RPS[pa2 + 1]])
                    tt('dve', tg1[:, 0:n], PS[pa2][:, 0:n], gtl[g_i][:, 0, 0:n], ALU.mult,
                       [RPS[pa2], R_gtl[g_i], R_tg], [R_tg])
                    tt('dve', tg2[:, 0:n], PS[pa2 + 1][:, 0:n], gtl[g_i][:, 1, 0:n], ALU.mult,
                       [RPS[pa2 + 1], R_gtl[g_i], R_tg], [R_tg])
                    tt('pool', GT[:, oc, 0:n], tg1[:, 0:n], tg2[:, 0:n], ALU.add, [R_tg], [R_GT])
                for tl_ in range(n // 128):
                    tk0 = t0 + tl_ * 128
                    sti = tl_
                    xi = xi_i[0] % 2
                    xi_i[0] += 1
                    dma('sp', xin[xi], x_in_ap(l, tk0, 128), [], [R_xin[xi]], key=KP_ld.next())
                    for hf in range(2):
                        pm = 6 + hf
                        for oc in range(8):
                            mm(PS[pm], GT[:, oc, tl_ * 128:(tl_ + 1) * 128], wo[:, oc, hf * 512:(hf + 1) * 512],
                               oc == 0, oc == 7, [R_GT, R_w3], [RPS[pm]])
                        tt('dve', yb[xi][:, hf * 512:(hf + 1) * 512], PS[pm], modbc[:, 0 + j, hf * 512:(hf + 1) * 512],
                           ALU.mult, [RPS[pm], R_modbc], [R_yb[xi]])
                    stt('dve', yb[xi], xin[xi], ALPHA, yb[xi], ALU.mult, ALU.add, [R_xin[xi], R_yb[xi]], [R_yb[xi]])
                    layer_norm_tile(yb[xi], R_yb[xi], 0, xs1[tk0:tk0 + 128, :], R_xs1)
                    for kh in range(2):
                        pb = 0 + kh
                        for k4 in range(4):
                            k = kh * 4 + k4
                            tr(PS[pb][:, k4 * 128:(k4 + 1) * 128], yb[xi][:, k * 128:(k + 1) * 128], [R_yb[xi]], [RPS[pb]])
                        for k4 in range(4):
                            k = kh * 4 + k4
                            act(hx2f[:, k, :], PS[pb][:, k4 * 128:(k4 + 1) * 128], AF.Identity, [RPS[pb], R_mod], [R_hx2f],
                                bias=modT[:, 3 * 8 + k, j:j + 1], scale=modT[:, 4 * 8 + k, j:j + 1])
                    cp('pool', hx2T[:, :, sti * 128:(sti + 1) * 128], hx2f, [R_hx2f], [R_hx2[sti]])
                    for k in range(8):
                        mm(PS[0][:, 0:16], hx2f[:, k, :], wr_sb[:, k, :], k == 0, k == 7, [R_hx2f, R_const], [RPS[0]])
                    lg = rt[:, 0:16]; pr = rt[:, 16:32]; pm6 = rt[:, 32:56]; gsc4 = rt[:, 56:60]
                    sc1 = rt[:, 60:61]; sc2 = rt[:, 61:62]; sc3 = rt[:, 62:63]; sc4 = rt[:, 63:64]
                    gm = rt[:, 64:68]; mk = rt[:, 68:84]; mk2 = rt[:, 84:100]; ok16 = rt[:, 100:116]
                    RR = [R_rt]
                    tt('dve', lg, PS[0][:, 0:16], brt_bc, ALU.add, [RPS[0], R_const, R_rt], RR)
                    S.op('dve', lambda e, lg=lg, sc1=sc1: e.reduce_max(out=sc1, in_=lg, axis=AX.X), RR, RR)
                    ts('dve', sc2, sc1, -1.0, ALU.mult, RR, RR)
                    memset('dve', sc3, 0.0, RR)
                    act(pr, lg, AF.Exp, RR, RR, bias=sc2, scale=1.0, accum=sc3)
                    recip(sc4, sc3, RR, RR)
                    ts('dve', pr, pr, sc4, ALU.mult, RR, RR)
                    pr4 = pr.rearrange('p (g e) -> p g e', e=4)
                    pm64 = pm6.rearrange('p (g s) -> p g s', s=6)
                    for si, (a_, b_) in enumerate([(0, 1), (0, 2), (0, 3), (1, 2), (1, 3), (2, 3)]):
                        tt('dve', pm64[:, :, si], pr4[:, :, a_], pr4[:, :, b_], ALU.add, RR, RR)
                    S.op('dve', lambda e, pm64=pm64, gsc4=gsc4: e.tensor_reduce(out=gsc4, in_=pm64, axis=AX.X, op=ALU.max),
                         RR, RR)
                    S.op('dve', lambda e, gsc4=gsc4, sc1=sc1: e.reduce_max(out=sc1, in_=gsc4, axis=AX.X), RR, RR)
                    ts('dve', gm, gsc4, sc1, ALU.is_ge, RR, RR)
                    ok3 = ok16.rearrange('p (g e) -> p g e', e=4)
                    for e4 in range(4):
                        cp('dve', ok3[:, :, e4], gm, RR, RR)
                    ts('dve', mk, pr, 1.0, ALU.add, RR, RR)
                    tt('dve', mk, mk, ok16, ALU.mult, RR, RR)
                    ts('dve', mk, mk, -1.0, ALU.add, RR, RR)
                    S.op('dve', lambda e, mk=mk, sc1=sc1: e.reduce_max(out=sc1, in_=mk, axis=AX.X), RR, RR)
                    ts('dve', mk2, mk, sc1, ALU.is_ge, RR, RR)
                    stt('dve', mk2, mk2, -2.0, mk, ALU.mult, ALU.add, RR, RR)
                    S.op('dve', lambda e, mk2=mk2, sc2=sc2: e.reduce_max(out=sc2, in_=mk2, axis=AX.X), RR, RR)
                    tt('dve', sc3, sc1, sc2, ALU.add, RR, RR)
                    recip(sc4, sc3, RR, RR)
                    ts('dve', mk2, mk, sc2, ALU.is_ge, RR, RR)
                    tt('dve', mk2, mk2, pr, ALU.mult, RR, RR)
                    ts('dve', gts[:, sti, :], mk2, sc4, ALU.mult, RR + [R_gts[sti]], [R_gts[sti]])
                S.barrier()
                A.release()
                if stop == 'C':
                    S.emit(); print('sched stats', S.stats); return nc
                A.mark()
                weg = [A.alloc([8, 512], BF16) for _ in range(2)]; weu = [A.alloc([8, 512], BF16) for _ in range(2)]
                wed = [A.alloc([4, D], BF16) for _ in range(2)]; R_we = [Res() for _ in range(2)]
                sg = [A.alloc([512], F32) for _ in range(2)]; R_sg = [Res() for _ in range(2)]
                hT = [A.alloc([4, 512], BF16) for _ in range(2)]; R_hT = [Res() for _ in range(2)]
                hres = [R_hx2[i] for i in range(n // 128)]
                for ex in range(NE):
                    wb = ex % 2
                    if not first_blk:
                        dma('sp', weg[wb].rearrange('p a d -> p (a d)'), wc_g[ex], [R_wc[ex]], [R_we[wb]], key=KP_ld.next())
                        dma('sp', weu[wb].rearrange('p a d -> p (a d)'), wc_u[ex], [R_wc[ex]], [R_we[wb]], key=KP_ld.next())
                        dma('sp', wed[wb].rearrange('p a d -> p (a d)'), wc_d[ex], [R_wc[ex]], [R_we[wb]], key=KP_ld.next())
                    for hh in (range(2) if first_blk else []):
                        load_cast(weg[wb][:, hh * 4:(hh + 1) * 4, :],
                                  w_e_gate[l, ex, hh * 512:(hh + 1) * 512, :].rearrange('(k p) c -> p k c', p=128), R_we[wb])
                    for hh in (range(2) if first_blk else []):
                        load_cast(weu[wb][:, hh * 4:(hh + 1) * 4, :],
                                  w_e_up[l, ex, hh * 512:(hh + 1) * 512, :].rearrange('(k p) c -> p k c', p=128), R_we[wb])
                    for hh in (range(2) if first_blk else []):
                        load_cast(wed[wb][:, :, hh * 512:(hh + 1) * 512],
                                  w_e_down[l, ex, :, hh * 512:(hh + 1) * 512].rearrange('(k p) c -> p k c', p=128), R_we[wb])
                    if first_blk:
                        dma('pool', wc_g[ex], weg[wb].rearrange('p a d -> p (a d)'), [R_we[wb]], [R_wc[ex]], key=KP_st.next())
                        dma('pool', wc_u[ex], weu[wb].rearrange('p a d -> p (a d)'), [R_we[wb]], [R_wc[ex]], key=KP_st.next())
                        dma('pool', wc_d[ex], wed[wb].rearrange('p a d -> p (a d)'), [R_we[wb]], [R_wc[ex]], key=KP_st.next())
                    hb_ = ex % 2
                    for dc in range(4):
                        pg = 2 + (dc % 2) * 2
                        for k in range(8):
                            mm(PS[pg][:, 0:n], weg[wb][:, k, dc * 128:(dc + 1) * 128], hx2T[:, k, 0:n], k == 0, k == 7,
                               [R_we[wb]] + hres, [RPS[pg]])
                        for k in range(8):
                            mm(PS[pg + 1][:, 0:n], weu[wb][:, k, dc * 128:(dc + 1) * 128], hx2T[:, k, 0:n], k == 0, k == 7,
                               [R_we[wb]] + hres, [RPS[pg + 1]])
                        si = dc % 2
                        act(sg[si][:, 0:n], PS[pg][:, 0:n], AF.Silu, [RPS[pg]], [R_sg[si]])
                        tt('dve', hT[hb_][:, dc, 0:n], sg[si][:, 0:n], PS[pg + 1][:, 0:n], ALU.mult,
                           [R_sg[si], RPS[pg + 1]], [R_hT[hb_]])
                    for tl_ in range(n // 128):
                        sti = tl_
                        for hf in range(2):
                            pm = 6 + hf
                            for dc in range(4):
                                mm(PS[pm], hT[hb_][:, dc, tl_ * 128:(tl_ + 1) * 128], wed[wb][:, dc, hf * 512:(hf + 1) * 512],
                                   dc == 0, dc == 3, [R_hT[hb_], R_we[wb]], [RPS[pm]])
                            if ex == 0:
                                ts('dve', macc[:, sti, hf * 512:(hf + 1) * 512], PS[pm], gts[:, sti, ex:ex + 1], ALU.mult,
                                   [RPS[pm], R_gts[sti]], [R_macc[sti]])
                            else:
                                stt('dve', macc[:, sti, hf * 512:(hf + 1) * 512], PS[pm], gts[:, sti, ex:ex + 1],
                                    macc[:, sti, hf * 512:(hf + 1) * 512], ALU.mult, ALU.add,
                                    [RPS[pm], R_gts[sti]], [R_macc[sti]])
                for tl_ in range(n // 128):
                    tk0 = t0 + tl_ * 128
                    sti = tl_
                    xi = xi_i[0] % 2
                    xi_i[0] += 1
                    dma('sp', xin[xi], xs1[tk0:tk0 + 128, :], [R_xs1], [R_xin[xi]], key=KP_ld.next())
                    tt('dve', yb[xi], macc[:, sti, :], modbc[:, 2 + j, :], ALU.mult, [R_macc[sti], R_modbc], [R_yb[xi]])
                    stt('dve', yb[xi], xin[xi], ALPHA, yb[xi], ALU.mult, ALU.add, [R_xin[xi], R_yb[xi]], [R_yb[xi]])
                    if last:
                        layer_norm_tile(yb[xi], R_yb[xi], 2, out[tk0:tk0 + 128, :], R_out)
                    else:
                        layer_norm_tile(yb[xi], R_yb[xi], 2, xs2[tk0:tk0 + 128, :], R_out)
                S.barrier()
                A.release()
                if stop == 'D':
                    S.emit(); print('sched stats', S.stats); return nc
            S.barrier()
            A.release()
            A.release()
            A.release()
            A.release()
        S.emit()
    print('sched stats', S.stats, 'arena peak', A.peak)
    return nc


def _perm_cols():
    cols = []
    for base in (0, 512):
        for h in range(4):
            prim, sw = [], []
            for m in range(2):
                o = base + h * 128 + m * 64
                p_ = [o + 2 * i for i in range(32)] + [o + 2 * i + 1 for i in range(32)]
                s_ = [o + 2 * i + 1 for i in range(32)] + [o + 2 * i for i in range(32)]
                prim += p_
                sw += s_
            cols += prim + sw
    cols += list(range(1024, 1536))
    for c in range(4):
        cols += list(range(1536 + c * 128, 1536 + (c + 1) * 128))
        cols += list(range(2048 + c * 128, 2048 + (c + 1) * 128))
    cols += list(range(2560, 4608))
    return np.array(cols, dtype=np.int64)


def _rope_tables(half):
    t = np.arange(half * NLAT, (half + 1) * NLAT)
    row = (t // 64).astype(np.float32)
    col = (t % 64).astype(np.float32)
    inv_freq = (np.float32(10000.0) ** (-np.arange(0, 32, 2, dtype=np.float32) / np.float32(32))).astype(np.float32)
    ang = np.concatenate([row[:, None] * inv_freq, col[:, None] * inv_freq], -1).astype(np.float32)
    c = np.cos(ang).astype(np.float32).T
    s = np.sin(ang).astype(np.float32).T
    C = np.concatenate([c, c, c, c], 0)
    Sg = np.concatenate([-s, s, -s, s], 0)
    return np.ascontiguousarray(C), np.ascontiguousarray(Sg)


_NC_CACHE = {}


def kernel(x, c, ctx, c_ctx, w_ada, b_ada, w_in, lam_q1, lam_k1, lam_q2, lam_k2, subln_g,
           w_attn_o, conv_w, conv_b, conv_ln_g, conv_ln_b, w_conv_o, w_out, ln1_g, ln1_b,
           w_router, b_router, w_e_gate, w_e_up, w_e_down, ln2_g, ln2_b, _dbg=False, _stop=None):
    f = lambda a: np.ascontiguousarray(np.asarray(a, dtype=np.float32))
    x, c, ctx, c_ctx = f(x), f(c), f(ctx), f(c_ctx)
    key = (bool(_dbg), _stop)
    if key not in _NC_CACHE:
        _NC_CACHE[key] = build_program(dbg=_dbg, stop=_stop)
    nc = _NC_CACHE[key]
    cols = _perm_cols()
    w_in_r = np.ascontiguousarray(f(w_in)[:, :, cols])
    b_adaT = np.ascontiguousarray(f(b_ada).reshape(2, 48, 128).transpose(2, 0, 1).reshape(128, 96))
    lamv = np.ascontiguousarray(np.stack([f(lam_q1), f(lam_k1), f(lam_q2), f(lam_k2)], 1).reshape(2, 256))
    convw = np.ascontiguousarray(f(conv_w).reshape(2, 31, 4, 128).transpose(3, 0, 2, 1).reshape(128, 248))
    convv = np.ascontiguousarray(np.stack([f(conv_b), f(conv_ln_g), f(conv_ln_b)], 1).reshape(2, 3, 4, 128)
                                 .transpose(3, 0, 1, 2).reshape(128, 24))
    lnv = np.ascontiguousarray(np.stack([f(ln1_g), f(ln1_b), f(ln2_g), f(ln2_b)], 1).reshape(2, 4 * D))
    w_router_r = np.ascontiguousarray(f(w_router).reshape(8, 128, 16).transpose(1, 0, 2).reshape(128, 128))
    shared = dict(identd=np.eye(128, dtype=np.float32), w_ada=f(w_ada), b_adaT=b_adaT, w_in_r=w_in_r, lamv=lamv,
                  sublng=f(subln_g), w_attn_o=f(w_attn_o), w_conv_o=f(w_conv_o), w_out=f(w_out), convw=convw,
                  convv=convv, lnv=lnv, w_router_r=w_router_r, b_router=f(b_router).reshape(1, 16),
                  w_e_gate=f(w_e_gate), w_e_up=f(w_e_up), w_e_down=f(w_e_down))
    if _stop in ('p0', 'A', 'X', 'B', 'C'):
        for kk in ('w_e_gate', 'w_e_up', 'w_e_down'):
            shared[kk] = np.ascontiguousarray(shared[kk][:, 0:1])
    ropes = [_rope_tables(h) for h in range(2)]
    in_maps = []
    for core in range(8):
        b, half = core // 2, core % 2
        cc = np.stack([c[b], c_ctx], 1)
        ccT = np.ascontiguousarray(cc.reshape(8, 128, 2).transpose(1, 0, 2).reshape(128, 16))
        hs = np.zeros((128, 2), np.float32)
        hs[:, 0] = 1.0 if half == 1 else 0.0
        hs[:, 1] = 1.0 if half == 0 else 0.0
        m = dict(shared)
        m.update(x_own=np.ascontiguousarray(x[b, half * NLAT:(half + 1) * NLAT]), ctxb=np.ascontiguousarray(ctx[b]),
                 ccT=ccT, ropeC=ropes[half][0], ropeS=ropes[half][1], halo_sel=hs)
        in_maps.append(m)
    res = run_bass_kernel_spmd(nc, in_maps, core_ids=list(range(8)))
    outp = np.empty((4, 8192, D), np.float32)
    for core in range(8):
        b, half = core // 2, core % 2
        outp[b, half * NLAT:(half + 1) * NLAT] = res.results[core]['out']
    if _dbg:
        return outp, res
    return outp
```
